# Optimizing a Trainium2 kernel written in Bass

```python
import jax
import jax.numpy as jnp
from jax import lax
import numpy as np

D_MODEL = 1024
BATCH = 32
SEQ = 2048
DEPTH = 4

N_MIXERS = 2
HEAD_SIZE = 64
N_HEADS = D_MODEL // HEAD_SIZE
DECAY_LORA = 64
ICLR_LORA = 64
VALUE_LORA = 32
GATE_LORA = 128
GN_EPS = HEAD_SIZE * 1e-5
CONV_WIDTH = 3
N_EXPERTS = 32
TOP_K = 4
D_FF = D_MODEL
SWIGLU_LIMIT = 7.0
SWIGLU_ALPHA = 1.702
BLOCK_ROWS = 256
PLE_DIM = 256
LN_EPS = 1e-5
DEEPNORM_ALPHA = (2.0 * DEPTH) ** 0.25
DEEPNORM_BETA = (8.0 * DEPTH) ** -0.25
N_RWKV = (DEPTH + N_MIXERS - 1) // N_MIXERS
N_CONV = DEPTH // N_MIXERS

kernel_name = 'rwkv7_shortconv_moe_deepnorm_hybrid'


def layer_norm(x, g, b):
    xf = x.astype(jnp.float32)
    mu = jnp.mean(xf, axis=-1, keepdims=True)
    var = jnp.mean(jnp.square(xf - mu), axis=-1, keepdims=True)
    return ((xf - mu) * lax.rsqrt(var + LN_EPS) * g + b).astype(x.dtype)


def token_shift(x):
    return jnp.pad(x, ((0, 0), (1, 0), (0, 0)))[:, :-1]


def wkv7_scan(r, decay, k, v, a, b):
    def step(S, inp):
        r_t, w_t, k_t, v_t, a_t, b_t = inp
        sa = jnp.einsum('bhij,bhj->bhi', S, a_t)
        S = S * w_t[:, :, None, :] + sa[..., None] * b_t[:, :, None, :] + v_t[..., None] * k_t[:, :, None, :]
        return S, jnp.einsum('bhij,bhj->bhi', S, r_t)
    seq = tuple(jnp.moveaxis(z.astype(jnp.float32), 1, 0) for z in (r, decay, k, v, a, b))
    bsz, _, h, n = r.shape
    s0 = jnp.zeros((bsz, h, n, n), jnp.float32)
    _, y = lax.scan(step, s0, seq)
    return jnp.moveaxis(y, 0, 1)


def rwkv7_time_mix(x, v_first, mix, w_rkv, w0, w1, w2, a0, a1, a2, g1, g2,
                   k_k, k_a, r_k, lnx_g, lnx_b, w_o, v_lora):
    bsz, t, d = x.shape
    xx = token_shift(x) - x
    x_rkv = x + xx * mix[:3, None, None, :]
    r, k, v = jnp.einsum('cbtd,cde->cbte', x_rkv, w_rkv)
    xw = x + xx * mix[3]
    xa = x + xx * mix[4]
    xg = x + xx * mix[5]
    w = -jax.nn.softplus(-(w0 + jnp.tanh(xw @ w1) @ w2).astype(jnp.float32)) - 0.5
    a = jax.nn.sigmoid(a0 + (xa @ a1) @ a2)
    g = jax.nn.sigmoid(xg @ g1) @ g2
    if v_lora is None:
        v_first = v
    else:
        v0, v1, v2 = v_lora
        v = v + (v_first - v) * jax.nn.sigmoid(v0 + (x_rkv[2] @ v1) @ v2)

    def heads(z):
        return z.reshape(bsz, t, N_HEADS, HEAD_SIZE)

    kk = heads(k * k_k).astype(jnp.float32)
    kk = kk / jnp.maximum(jnp.sqrt(jnp.sum(kk * kk, axis=-1, keepdims=True)), 1e-12)
    k = k * (1 + (a - 1) * k_a)
    r, k, v, a = heads(r), heads(k), heads(v), heads(a).astype(jnp.float32)
    decay = jnp.exp(-jnp.exp(heads(w)))
    y = wkv7_scan(r, decay, k, v, -kk, kk * a)
    mu = jnp.mean(y, axis=-1, keepdims=True)
    var = jnp.mean(jnp.square(y - mu), axis=-1, keepdims=True)
    y = ((y - mu) * lax.rsqrt(var + GN_EPS)).reshape(bsz, t, d) * lnx_g + lnx_b
    bonus = jnp.sum(r.astype(jnp.float32) * k.astype(jnp.float32) * r_k, axis=-1, keepdims=True) * v.astype(jnp.float32)
    y = (y + bonus.reshape(bsz, t, d)).astype(x.dtype)
    return (y * g) @ w_o, v_first


def short_conv_mix(x, w_in, conv_w, w_out):
    d = x.shape[-1]
    gate_b, gate_c, h = jnp.split(x @ w_in, 3, axis=-1)
    u = lax.conv_general_dilated(gate_c * h, conv_w[:, None, :], window_strides=(1,),
                                 padding=[(CONV_WIDTH - 1, 0)],
                                 dimension_numbers=('NWC', 'WIO', 'NWC'),
                                 feature_group_count=d)
    return (gate_b * u) @ w_out


def moe_ffn(x2, router_w, router_b, w_gu, b_gu, w_down, b_down):
    n, d = x2.shape
    logits = x2.astype(jnp.float32) @ router_w.astype(jnp.float32) + router_b.astype(jnp.float32)
    top_val, top_idx = lax.top_k(logits, TOP_K)
    gate = jax.nn.softmax(top_val, axis=-1)
    n_assign = n * TOP_K
    flat_e = top_idx.reshape(n_assign)
    order = jnp.argsort(flat_e)
    sorted_e = flat_e[order]
    sorted_tok = order // TOP_K
    sorted_gate = gate.reshape(n_assign)[order]
    counts = jnp.bincount(flat_e, length=N_EXPERTS)
    padded = (counts + BLOCK_ROWS - 1) // BLOCK_ROWS * BLOCK_ROWS
    pad_end = jnp.cumsum(padded)
    pad_start = pad_end - padded
    start = jnp.cumsum(counts) - counts
    dest = pad_start[sorted_e] + jnp.arange(n_assign, dtype=jnp.int32) - start[sorted_e]
    n_blocks = -(-n_assign // BLOCK_ROWS) + N_EXPERTS
    rows = n_blocks * BLOCK_ROWS
    buf = jnp.zeros((rows, d), x2.dtype).at[dest].set(x2[sorted_tok])
    block_e = jnp.minimum(jnp.searchsorted(pad_end, jnp.arange(n_blocks) * BLOCK_ROWS, side='right'),
                          N_EXPERTS - 1)

    def expert_block(args):
        xb, e = args
        hcat = xb @ w_gu[e] + b_gu[e]
        glu = jnp.minimum(hcat[:, :D_FF], SWIGLU_LIMIT)
        lin = jnp.clip(hcat[:, D_FF:], -SWIGLU_LIMIT, SWIGLU_LIMIT)
        act = glu * jax.nn.sigmoid(SWIGLU_ALPHA * glu) * (lin + 1)
        return act @ w_down[e] + b_down[e]

    out = lax.map(expert_block, (buf.reshape(n_blocks, BLOCK_ROWS, d), block_e)).reshape(rows, d)
    contrib = out[dest] * sorted_gate[:, None].astype(out.dtype)
    return jax.ops.segment_sum(contrib, sorted_tok, num_segments=n)


def setup_inputs(seed: int = 0) -> dict:
    key = jax.random.key(seed)
    ks = iter(jax.random.split(key, 48))

    def nrm(shape, scale):
        return scale * jax.random.normal(next(ks), shape, jnp.float32)

    def uni(shape, lo, hi):
        return jax.random.uniform(next(ks), shape, jnp.float32, lo, hi)

    D, E, F, NR, NC, L = D_MODEL, N_EXPERTS, D_FF, N_RWKV, N_CONV, DEPTH
    fan = D ** -0.5
    beta = DEEPNORM_BETA
    return {
        'x': nrm((BATCH, SEQ, D), 1.0),
        'p': nrm((L, BATCH, SEQ, PLE_DIM), 1.0),
        'rwkv_mix': uni((NR, 6, D), 0.0, 1.0),
        'rwkv_w_rkv': nrm((NR, 3, D, D), fan) * jnp.array([1.0, 1.0, beta], jnp.float32)[None, :, None, None],
        'rwkv_w0': uni((NR, D), -6.0, 1.0),
        'rwkv_w1': nrm((NR, D, DECAY_LORA), fan),
        'rwkv_w2': nrm((NR, DECAY_LORA, D), 0.5 * DECAY_LORA ** -0.5),
        'rwkv_a0': nrm((NR, D), 0.1),
        'rwkv_a1': nrm((NR, D, ICLR_LORA), fan),
        'rwkv_a2': nrm((NR, ICLR_LORA, D), 0.5 * ICLR_LORA ** -0.5),
        'rwkv_v0': 1.0 + nrm((NR - 1, D), 0.1),
        'rwkv_v1': nrm((NR - 1, D, VALUE_LORA), fan),
        'rwkv_v2': nrm((NR - 1, VALUE_LORA, D), 0.5 * VALUE_LORA ** -0.5),
        'rwkv_g1': nrm((NR, D, GATE_LORA), fan),
        'rwkv_g2': nrm((NR, GATE_LORA, D), GATE_LORA ** -0.5),
        'rwkv_k_k': 0.85 + nrm((NR, D), 0.05),
        'rwkv_k_a': 1.0 + nrm((NR, D), 0.05),
        'rwkv_r_k': nrm((NR, N_HEADS, HEAD_SIZE), 0.1),
        'rwkv_lnx_g': 1.0 + nrm((NR, D), 0.05),
        'rwkv_lnx_b': nrm((NR, D), 0.02),
        'rwkv_w_o': nrm((NR, D, D), fan * beta),
        'conv_w_in': nrm((NC, D, 3 * D), fan),
        'conv_w': nrm((NC, CONV_WIDTH, D), CONV_WIDTH ** -0.5),
        'conv_w_out': nrm((NC, D, D), fan * beta),
        'ln_mix_g': 1.0 + nrm((L, D), 0.05),
        'ln_mix_b': nrm((L, D), 0.02),
        'router_w': nrm((L, D, E), fan),
        'router_b': nrm((L, E), 0.01),
        'moe_w_gu': nrm((L, E, D, 2 * F), fan),
        'moe_b_gu': nrm((L, E, 2 * F), 0.01),
        'moe_w_down': nrm((L, E, F, D), F ** -0.5 * beta),
        'moe_b_down': nrm((L, E, D), 0.01),
        'ln_ffn_g': 1.0 + nrm((L, D), 0.05),
        'ln_ffn_b': nrm((L, D), 0.02),
        'ple_w_proj': nrm((L, PLE_DIM, D), PLE_DIM ** -0.5 * beta),
        'ple_w_gate': nrm((L, D, D), fan),
        'ple_b_gate': nrm((L, D), 0.01),
        'ln_ple_g': 1.0 + nrm((L, D), 0.05),
        'ln_ple_b': nrm((L, D), 0.02),
    }


def reference(x, p, rwkv_mix, rwkv_w_rkv, rwkv_w0, rwkv_w1, rwkv_w2, rwkv_a0, rwkv_a1, rwkv_a2,
              rwkv_v0, rwkv_v1, rwkv_v2, rwkv_g1, rwkv_g2, rwkv_k_k, rwkv_k_a, rwkv_r_k,
              rwkv_lnx_g, rwkv_lnx_b, rwkv_w_o, conv_w_in, conv_w, conv_w_out,
              ln_mix_g, ln_mix_b, router_w, router_b, moe_w_gu, moe_b_gu, moe_w_down, moe_b_down,
              ln_ffn_g, ln_ffn_b, ple_w_proj, ple_w_gate, ple_b_gate, ln_ple_g, ln_ple_b):
    bsz, t, d = x.shape
    v_first = None
    for i in range(DEPTH):
        j = i // N_MIXERS
        if i % N_MIXERS == 0:
            v_lora = None if j == 0 else (rwkv_v0[j - 1], rwkv_v1[j - 1], rwkv_v2[j - 1])
            mixed, v_first = rwkv7_time_mix(
                x, v_first, rwkv_mix[j], rwkv_w_rkv[j], rwkv_w0[j], rwkv_w1[j], rwkv_w2[j],
                rwkv_a0[j], rwkv_a1[j], rwkv_a2[j], rwkv_g1[j], rwkv_g2[j], rwkv_k_k[j], rwkv_k_a[j],
                rwkv_r_k[j], rwkv_lnx_g[j], rwkv_lnx_b[j], rwkv_w_o[j], v_lora)
        else:
            mixed = short_conv_mix(x, conv_w_in[j], conv_w[j], conv_w_out[j])
        x = layer_norm(DEEPNORM_ALPHA * x + mixed, ln_mix_g[i], ln_mix_b[i])
        ffn = moe_ffn(x.reshape(bsz * t, d), router_w[i], router_b[i], moe_w_gu[i], moe_b_gu[i],
                      moe_w_down[i], moe_b_down[i]).reshape(bsz, t, d)
        x = layer_norm(DEEPNORM_ALPHA * x + ffn, ln_ffn_g[i], ln_ffn_b[i])
        ple = (p[i] @ ple_w_proj[i]) * jax.nn.sigmoid(x @ ple_w_gate[i] + ple_b_gate[i])
        x = layer_norm(DEEPNORM_ALPHA * x + ple, ln_ple_g[i], ln_ple_b[i])
    return x
```

```python
import numpy as np
from contextlib import ExitStack
import concourse.bass as bass
import concourse.mybir as mybir
from concourse.bass_utils import run_bass_kernel_spmd

F32 = mybir.dt.float32
BF16 = mybir.dt.bfloat16
ALU = mybir.AluOpType
AF = mybir.ActivationFunctionType

D = 1024
KC = 8
P = 128
HS = 64
PLE = 256
LN_EPS = 1e-5
GN_EPS = 64 * 1e-5
TOPK = 4


class Cfg:
    def __init__(self, T=2048, NB=4, E=32, L=4):
        self.T, self.NB, self.E, self.L = T, NB, E, L
        self.NTOK = T * NB
        self.alpha = (2.0 * L) ** 0.25
        self.NR = (L + 1) // 2
        self.NCV = L // 2


class Sem:
    def __init__(self, h, name):
        self.h, self.name, self.total = h, name, 0


class Buf:
    __slots__ = ("name", "w", "r", "sem", "dram")

    def __init__(self, name, dram=False):
        self.name, self.w, self.r, self.sem, self.dram = name, None, [], None, dram


class Eng:
    def __init__(self, key, sem):
        self.key, self.sem, self.count, self.waited, self.ops = key, sem, 0, {}, []


class Sched:
    def __init__(self, nc, stack, nsem=100):
        self.nc = nc
        self.sems = [Sem(stack.enter_context(nc.semaphore(f"s{i}")), f"s{i}") for i in range(nsem)]
        self.free = list(self.sems)
        self.eng = {k: Eng(k, self.free.pop()) for k in ("pe", "dve", "act", "pool", "sp")}
        self.dmabufs = []

    def _deps(self, e, reads, writes, dma_sem=None):
        deps = {}

        def add(d):
            if d is None:
                return
            s, c = d
            if (s is e.sem and e.key == "pe") or s is dma_sem:
                return
            if deps.get(s, 0) < c:
                deps[s] = c

        for b in reads:
            add(b.w)
        for b in writes:
            if b.dram:
                continue
            add(b.w)
            for d in b.r:
                add(d)
        out = []
        for s, c in deps.items():
            if e.waited.get(s, 0) < c:
                e.waited[s] = c
                out.append((s.h, c))
        return out

    def op(self, ek, fn, reads=(), writes=()):
        e = self.eng[ek]
        waits = self._deps(e, reads, writes)
        e.count += 1
        sem_h = e.sem.h

        def thunk(h):
            for s, c in waits:
                h.wait_ge(s, c)
            fn(h).then_inc(sem_h, 1)

        e.ops.append(thunk)
        me = (e.sem, e.count)
        for b in reads:
            b.r = [d for d in b.r if d[0] is not e.sem] + [me]
        for b in writes:
            b.w = me
            b.r = []

    def dma(self, qk, out_ap, in_ap, reads=(), writes=()):
        e = self.eng[qk]
        b = writes[0]
        if b.sem is None:
            b.sem = self.free.pop()
            self.dmabufs.append(b)
        waits = self._deps(e, reads, writes, dma_sem=b.sem)
        b.sem.total += 16
        sem_h = b.sem.h

        def thunk(h):
            for s, c in waits:
                h.wait_ge(s, c)
            h.dma_start(out=out_ap, in_=in_ap).then_inc(sem_h, 16)

        e.ops.append(thunk)
        me = (b.sem, b.sem.total)
        for rb in reads:
            rb.r = [d for d in rb.r if d[0] is not b.sem] + [me]
        for wb in writes:
            wb.w = me
            wb.r = []

    def run_phase(self, name):
        nc = self.nc
        sp = self.eng["sp"]
        fin = []
        for b in self.dmabufs:
            if sp.waited.get(b.sem, 0) < b.sem.total:
                sp.waited[b.sem] = b.sem.total
                fin.append((b.sem.h, b.sem.total))
        for k in ("pe", "dve", "act", "pool"):
            e = self.eng[k]
            if e.count and sp.waited.get(e.sem, 0) < e.count:
                sp.waited[e.sem] = e.count
                fin.append((e.sem.h, e.count))

        def spfin(h):
            for s, c in fin:
                h.wait_ge(s, c)

        sp.ops.append(spfin)
        with nc.Block() as block:
            for k, dec in (("sp", block.sync), ("pe", block.tensor), ("dve", block.vector),
                           ("act", block.scalar), ("pool", block.gpsimd)):
                ops = self.eng[k].ops
                if not ops:
                    continue

                def body(h, ops=ops):
                    for t in ops:
                        t(h)

                dec(body)
        for e in self.eng.values():
            e.ops = []
        for b in self.dmabufs:
            self.free.append(b.sem)
            b.sem, b.w, b.r = None, None, []
        self.dmabufs = []


class Builder:
    def __init__(self, cfg):
        self.cfg = cfg
        self.nc = bass.Bass("TRN2", target_bir_lowering=False)
        self.din = {}
        self.uid = 0

    def inp(self, name, shape, dt=F32):
        t = self.nc.dram_tensor(name, list(shape), dt, kind="ExternalInput").ap()
        self.din[name] = t
        return t

    def un(self, name):
        self.uid += 1
        return f"{name}_{self.uid}"

    def scratch(self, name, shape, dt=F32):
        return self.nc.dram_tensor(name, list(shape), dt, kind="Internal").ap()

    def mm(self, out, lhsT, rhs, start, stop, reads, writes):
        self.S.op("pe", lambda h: h.matmul(out, lhsT, rhs, start=start, stop=stop), reads, writes)

    def tr(self, out, in_, ident, reads, writes):
        self.S.op("pe", lambda h: h.transpose(out, in_, ident), reads, writes)

    def tt(self, ek, out, in0, in1, op, reads, writes):
        self.S.op(ek, lambda h: h.tensor_tensor(out=out, in0=in0, in1=in1, op=op), reads, writes)

    def ts(self, ek, out, in0, s1, s2, op0, op1, reads, writes):
        if op1 is None:
            self.S.op(ek, lambda h: h.tensor_scalar(out=out, in0=in0, scalar1=s1, scalar2=None, op0=op0),
                      reads, writes)
        else:
            self.S.op(ek, lambda h: h.tensor_scalar(out=out, in0=in0, scalar1=s1, scalar2=s2, op0=op0, op1=op1),
                      reads, writes)

    def stt(self, out, in0, scalar, in1, op0, op1, reads, writes):
        self.S.op("dve", lambda h: h.scalar_tensor_tensor(out=out, in0=in0, scalar=scalar, in1=in1,
                                                          op0=op0, op1=op1), reads, writes)

    def act(self, out, in_, func, reads, writes, bias=None, scale=None):
        kw = {}
        if bias is not None:
            kw["bias"] = bias
        if scale is not None:
            kw["scale"] = scale
        self.S.op("act", lambda h: h.activation(out=out, in_=in_, func=func, **kw), reads, writes)

    def cp(self, ek, out, in_, reads, writes):
        if ek == "act":
            self.S.op("act", lambda h: h.copy(out=out, in_=in_), reads, writes)
        else:
            self.S.op(ek, lambda h: h.tensor_copy(out=out, in_=in_), reads, writes)

    def memset(self, ek, ap, val, writes):
        self.S.op(ek, lambda h: h.memset(ap, val), (), writes)

    def ld(self, out, in_, buf, q="sp", extra_reads=()):
        self.S.dma(q, out, in_, extra_reads, [buf])

    def st(self, out, in_, dbuf, rbufs, q="sp"):
        self.S.dma(q, out, in_, rbufs, [dbuf])

    def build(self):
        cfg, nc = self.cfg, self.nc
        NTOK, L, E = cfg.NTOK, cfg.L, cfg.E
        NR, NCV = cfg.NR, cfg.NCV
        x_in = self.inp("x", [NTOK, D])
        p_in = self.inp("p", [L, NTOK, PLE])
        cst = self.inp("cst", [P, 4 * P])
        blk2_in = self.inp("blk2", [P, 2])
        NVR = 6 * 8 + 9 * 8
        vr_in = self.inp("vec_rwkv", [NR, P, NVR])
        vc_in = self.inp("vec_conv", [max(NCV, 1), P, 3 * 8])
        NVL = 7 * 8
        vl_in = self.inp("vec_layer", [L, P, NVL])
        bgu_in = self.inp("b_gu", [L, P, E * 16])
        bdn_in = self.inp("b_down", [L, E, D])
        rb_in = self.inp("router_b", [L, 1, E])
        rw_in = self.inp("router_w", [L, D, E])
        wrkv_in = self.inp("w_rkv", [NR, 3, D, D])
        w1_in = self.inp("w1", [NR, D, 64]); w2_in = self.inp("w2", [NR, 64, D])
        a1_in = self.inp("a1", [NR, D, 64]); a2_in = self.inp("a2", [NR, 64, D])
        g1_in = self.inp("g1", [NR, D, 128]); g2_in = self.inp("g2", [NR, 128, D])
        v1_in = self.inp("v1", [max(NR - 1, 1), D, 32]); v2_in = self.inp("v2", [max(NR - 1, 1), 32, D])
        wo_in = self.inp("w_o", [NR, D, D])
        cwin_in = self.inp("conv_w_in", [max(NCV, 1), D, 3 * D])
        cwout_in = self.inp("conv_w_out", [max(NCV, 1), D, D])
        wgu_in = self.inp("moe_w_gu", [L, E, D, 2 * D])
        wdn_in = self.inp("moe_w_down", [L, E, D, D])
        pproj_in = self.inp("ple_w_proj", [L, PLE, D])
        pgate_in = self.inp("ple_w_gate", [L, D, D])
        y_out = nc.dram_tensor("y", [NTOK, D], F32, kind="ExternalOutput").ap()
        XA = self.scratch("XA", [KC, P, NTOK]); XB = self.scratch("XB", [KC, P, NTOK])
        Rs = self.scratch("Rs", [KC, P, NTOK]); Ws = self.scratch("Ws", [KC, P, NTOK])
        Ks = self.scratch("Ks", [KC, P, NTOK]); As = self.scratch("As", [KC, P, NTOK])
        Bs = self.scratch("Bs", [KC, P, NTOK]); Gs = self.scratch("Gs", [KC, P, NTOK])
        BONs = self.scratch("BONs", [KC, P, NTOK]); VFs = self.scratch("VFs", [KC, P, NTOK])
        Ys = self.scratch("Ys", [KC, P, NTOK])
        Vtok = self.scratch("Vtok", [NTOK, D], BF16)
        FFN = self.scratch("FFN", [NTOK, D])
        self.dr = dict(XA=XA, XB=XB)
        self.y_out = y_out

        with ExitStack() as gs:
            self.S = Sched(nc, gs)
            S = self.S
            ps = gs.enter_context(nc.psum_tensor("ps", [P, 4096], F32))
            self.ps = ps
            self.pb = [Buf(f"bank{i}") for i in range(8)]
            cst_t = gs.enter_context(nc.sbuf_tensor("cst_t", [P, 4 * P], F32))
            blk2f = gs.enter_context(nc.sbuf_tensor("blk2f", [P, 2], F32))
            blk2b = gs.enter_context(nc.sbuf_tensor("blk2b", [P, 2], BF16))
            blkb = gs.enter_context(nc.sbuf_tensor("blkb", [P, P], BF16))
            identb = gs.enter_context(nc.sbuf_tensor("identb", [P, P], BF16))
            self.ident = cst_t[:, 0:P]; self.onesD = cst_t[:, P:2 * P]
            self.blk = cst_t[:, 2 * P:3 * P]; self.blk64 = cst_t[:, 3 * P:4 * P]
            self.blkb, self.blk2b, self.identb = blkb, blk2b, identb
            self.cbuf = Buf("cst")
            self.ld(cst_t[:], cst[:, :], self.cbuf)
            self.ld(blk2f[:], blk2_in[:, :], self.cbuf)
            self.cp("dve", blk2b[:], blk2f[:], [self.cbuf], [self.cbuf])
            self.cp("dve", blkb[:], self.blk, [self.cbuf], [self.cbuf])
            self.cp("dve", identb[:], self.ident, [self.cbuf], [self.cbuf])
            S.run_phase("init")

            self.phase_in(x_in, XA)
            cur, nxt = XA, XB
            stop = getattr(cfg, "stop", 10 ** 9)
            nsub = 0
            for i in range(L):
                j = i // 2
                if nsub >= stop:
                    break
                if i % 2 == 0:
                    self.phase_rwkv_proj(cur, j, vr_in, wrkv_in, w1_in, w2_in, a1_in, a2_in, g1_in, g2_in,
                                         v1_in, v2_in, Rs, Ws, Ks, As, Bs, Gs, BONs, VFs, Vtok)
                    self.phase_scan(Rs, Ws, Ks, As, Bs, Vtok, Ys)
                    self.phase_rwkv_out(cur, nxt, j, i, vr_in, vl_in, wo_in, Ys, Gs, BONs)
                else:
                    self.phase_conv(cur, nxt, j, i, vc_in, vl_in, cwin_in, cwout_in)
                cur, nxt = nxt, cur
                nsub += 1
                if nsub >= stop:
                    break
                self.phase_moe(cur, i, rw_in, rb_in, wgu_in, wdn_in, bgu_in, bdn_in, FFN)
                if getattr(cfg, "debug", None) == "gate":
                    self.end_phase("dbg")
                    return nc
                if getattr(cfg, "debug", None) == "ffn":
                    db = Buf("dbg", dram=True)
                    self.S.dma("sp", y_out[:, :], FFN[:, :], (), [db])
                    self.end_phase("dbg")
                    return nc
                self.phase_moe_epi(cur, nxt, i, vl_in, FFN)
                cur, nxt = nxt, cur
                nsub += 1
                if nsub >= stop:
                    break
                self.phase_ple(cur, nxt, i, vl_in, p_in, pproj_in, pgate_in)
                cur, nxt = nxt, cur
                nsub += 1
            self.phase_out(cur, y_out)
        return nc

    def end_phase(self, name):
        self.S.run_phase(name)
        for b in self.pb:
            b.w, b.r = None, []
        self.cbuf.w, self.cbuf.r = None, []

    def bkn(self, i, n):
        return self.ps[:, i * 512:i * 512 + n]

    def bank(self, i):
        return self.ps[:, i * 512:(i + 1) * 512]

    def phase_in(self, x_in, XA):
        cfg, nc = self.cfg, self.nc
        with ExitStack() as st:
            xt = [st.enter_context(nc.sbuf_tensor(self.un(f"pi_xt{i}"), [P, 4, D], F32)) for i in range(2)]
            xf = [st.enter_context(nc.sbuf_tensor(self.un(f"pi_xf{i}"), [P, KC, 512], F32)) for i in range(2)]
            xtb = [Buf(f"xt{i}") for i in range(2)]
            xfb = [Buf(f"xf{i}") for i in range(2)]
            dst = Buf("XA", dram=True)
            for g in range(cfg.NTOK // 512):
                s = g % 2
                self.ld(xt[s][:], x_in[g * 512:(g + 1) * 512, :].rearrange("(a p) d -> p a d", p=P), xtb[s])
                for c in range(KC):
                    bk = (g * KC + c) % 8
                    for a in range(4):
                        self.tr(self.ps[:, bk * 512 + a * P: bk * 512 + (a + 1) * P],
                                xt[s][:, a, c * P:(c + 1) * P], self.ident, [xtb[s], self.cbuf], [self.pb[bk]])
                    self.cp("dve" if c % 2 == 0 else "act", xf[s][:, c, :], self.bank(bk), [self.pb[bk]], [xfb[s]])
                self.st(XA[:, :, g * 512:(g + 1) * 512].rearrange("c p n -> p c n"), xf[s][:], dst, [xfb[s]])
            self.end_phase("in")

    def phase_out(self, X, y_out):
        cfg, nc = self.cfg, self.nc
        with ExitStack() as st:
            xf = [st.enter_context(nc.sbuf_tensor(self.un(f"po_xf{i}"), [P, KC, 512], F32)) for i in range(2)]
            xt = [st.enter_context(nc.sbuf_tensor(self.un(f"po_xt{i}"), [P, 4, D], F32)) for i in range(2)]
            xtb = [Buf(f"xt{i}") for i in range(2)]
            xfb = [Buf(f"xf{i}") for i in range(2)]
            dst = Buf("Y", dram=True)
            for g in range(cfg.NTOK // 512):
                s = g % 2
                self.ld(xf[s][:], X[:, :, g * 512:(g + 1) * 512].rearrange("c p n -> p c n"), xfb[s])
                for a in range(4):
                    for hf in range(2):
                        bk = (g * 8 + a * 2 + hf) % 8
                        for cc in range(4):
                            c = hf * 4 + cc
                            self.tr(self.ps[:, bk * 512 + cc * P: bk * 512 + (cc + 1) * P],
                                    xf[s][:, c, a * P:(a + 1) * P], self.ident, [xfb[s], self.cbuf], [self.pb[bk]])
                        self.cp("dve" if hf == 0 else "act", xt[s][:, a, hf * 512:(hf + 1) * 512], self.bank(bk),
                                [self.pb[bk]], [xtb[s]])
                self.st(y_out[g * 512:(g + 1) * 512, :].rearrange("(a p) d -> p a d", p=P), xt[s][:], dst, [xtb[s]])
            self.end_phase("out")

    def ln_fm(self, z, zb, N, g_ap, b_ap, tmp, tmpb, bk0, vb):
        sq, mean, rstd = tmp["sq"], tmp["mean"], tmp["rstd"]
        b_mean, b_msq = bk0, bk0 + 1
        self.act(sq[:, :, 0:N], z[:, :, 0:N], AF.Square, [zb], [tmpb["sq"]])
        for c in range(KC):
            self.mm(self.ps[:, b_mean * 512:b_mean * 512 + N], self.onesD, z[:, c, 0:N], c == 0, c == KC - 1,
                    [zb, self.cbuf], [self.pb[b_mean]])
        for c in range(KC):
            self.mm(self.ps[:, b_msq * 512:b_msq * 512 + N], self.onesD, sq[:, c, 0:N], c == 0, c == KC - 1,
                    [tmpb["sq"], self.cbuf], [self.pb[b_msq]])
        pm = self.ps[:, b_mean * 512:b_mean * 512 + N]
        pq = self.ps[:, b_msq * 512:b_msq * 512 + N]
        self.cp("act", mean[:, 0:N], pm, [self.pb[b_mean]], [tmpb["mean"]])
        self.tt("dve", rstd[:, 0:N], mean[:, 0:N], mean[:, 0:N], ALU.mult, [tmpb["mean"]], [tmpb["rstd"]])
        self.tt("dve", rstd[:, 0:N], pq, rstd[:, 0:N], ALU.subtract, [self.pb[b_msq], tmpb["rstd"]], [tmpb["rstd"]])
        self.ts("dve", rstd[:, 0:N], rstd[:, 0:N], 0.0, LN_EPS, ALU.max, ALU.add, [tmpb["rstd"]], [tmpb["rstd"]])
        self.act(rstd[:, 0:N], rstd[:, 0:N], AF.Sqrt, [tmpb["rstd"]], [tmpb["rstd"]])
        self.S.op("dve", lambda h: h.reciprocal(out=rstd[:, 0:N], in_=rstd[:, 0:N]), [tmpb["rstd"]], [tmpb["rstd"]])
        for c in range(KC):
            self.tt("dve", z[:, c, 0:N], z[:, c, 0:N], mean[:, 0:N], ALU.subtract, [zb, tmpb["mean"]], [zb])
            self.tt("dve", z[:, c, 0:N], z[:, c, 0:N], rstd[:, 0:N], ALU.mult, [zb, tmpb["rstd"]], [zb])
            self.act(z[:, c, 0:N], z[:, c, 0:N], AF.Identity, [zb, vb], [zb],
                     bias=b_ap[:, c:c + 1], scale=g_ap[:, c:c + 1])

    def ln_tmp(self, st, N, tag):
        nc = self.nc
        tmp = dict(sq=st.enter_context(nc.sbuf_tensor(self.un(f"{tag}_sq"), [P, KC, N], F32)),
                   mean=st.enter_context(nc.sbuf_tensor(self.un(f"{tag}_mean"), [P, N], F32)),
                   rstd=st.enter_context(nc.sbuf_tensor(self.un(f"{tag}_rstd"), [P, N], F32)))
        tmpb = dict(sq=Buf("sq"), mean=Buf("mean"), rstd=Buf("rstd"))
        return tmp, tmpb

    def wload(self, dst_tile, src_ap, buf, q="pool"):
        self.S.dma(q, dst_tile, src_ap, (), [buf])

    def phase_rwkv_proj(self, X, j, vr_in, wrkv_in, w1_in, w2_in, a1_in, a2_in, g1_in, g2_in, v1_in, v2_in,
                        Rs, Ws, Ks, As, Bs, Gs, BONs, VFs, Vtok):
        cfg, nc = self.cfg, self.nc
        T = cfg.T
        N = 256
        NS = N // P
        with ExitStack() as st:
            sb = lambda name, shape, dt=F32: st.enter_context(nc.sbuf_tensor(self.un(name), shape, dt))
            vec = sb("rp_vec", [P, 6 * 8 + 9 * 8]); vb = Buf("vec")
            self.ld(vec[:], vr_in[j], vb)
            mixv = lambda c: vec[:, c * 8:(c + 1) * 8]
            voff = lambda k: vec[:, 48 + k * 8: 48 + (k + 1) * 8]
            w0v, a0v, v0v, kkv, kav, rkv = (voff(k) for k in range(6))
            omka = sb("rp_omka", [P, 8])
            self.ts("dve", omka[:], kav, -1.0, 1.0, ALU.mult, ALU.add, [vb], [vb])
            wq = sb("rp_wrkv", [P, 3, KC, D], BF16); wqb = Buf("wrkv")
            for c in range(3):
                self.wload(wq[:, c], wrkv_in[j, c].rearrange("(k p) o -> p k o", p=P), wqb)
            w1 = sb("rp_w1", [P, KC, 64], BF16); a1 = sb("rp_a1", [P, KC, 64], BF16)
            g1 = sb("rp_g1", [P, KC, 128], BF16); v1 = sb("rp_v1", [P, KC, 32], BF16)
            w2 = sb("rp_w2", [64, D], BF16); a2 = sb("rp_a2", [64, D], BF16)
            g2 = sb("rp_g2", [P, D], BF16); v2 = sb("rp_v2", [32, D], BF16)
            wsb = Buf("wsmall")
            self.wload(w1[:], w1_in[j].rearrange("(k p) o -> p k o", p=P), wsb)
            self.wload(a1[:], a1_in[j].rearrange("(k p) o -> p k o", p=P), wsb)
            self.wload(g1[:], g1_in[j].rearrange("(k p) o -> p k o", p=P), wsb)
            self.wload(w2[:], w2_in[j], wsb); self.wload(a2[:], a2_in[j], wsb); self.wload(g2[:], g2_in[j], wsb)
            if j > 0:
                self.wload(v1[:], v1_in[j - 1].rearrange("(k p) o -> p k o", p=P), wsb)
                self.wload(v2[:], v2_in[j - 1], wsb)
            xc = [sb(f"rp_x{i}", [P, KC, N]) for i in range(2)]
            xp = [sb(f"rp_xp{i}", [P, KC, N]) for i in range(2)]
            xcb = [Buf("xc0"), Buf("xc1")]; xpb = [Buf("xp0"), Buf("xp1")]
            xx = sb("rp_xx", [P, KC, N]); xxb = Buf("xx")
            xm = [sb(f"rp_xm{c}", [P, KC, N], BF16) for c in range(6)]
            xmb = [Buf(f"xm{c}") for c in range(6)]
            hw = sb("rp_hw", [64, N], BF16); ha = sb("rp_ha", [64, N], BF16)
            hg = sb("rp_hg", [P, N], BF16); hv = sb("rp_hv", [32, N], BF16)
            hb = [Buf("hw"), Buf("ha"), Buf("hg"), Buf("hv")]
            nm = ["r", "k", "v", "dec", "a", "kk", "t1", "t2", "g", "vf", "G", "ig", "gp"]
            tl = {n: [sb(f"rp_{n}{i}", [P, N]) for i in range(2)] for n in nm}
            tb = {n: [Buf(f"{n}{i}") for i in range(2)] for n in nm}
            vtb16 = [sb(f"rp_vt{i}", [P, NS, D], BF16) for i in range(2)]; vtb = [Buf("vt0"), Buf("vt1")]
            vfm = [sb(f"rp_vfm{i}", [P, KC, N], BF16) for i in range(2)]; vfmb = [Buf("vfm0"), Buf("vfm1")]
            cmask = sb("rp_cmask", [P, N]); cmb = Buf("cmask")
            self.memset("dve", cmask[:], 1.0, [cmb])
            self.memset("dve", cmask[:].rearrange("p (a b) -> p a b", b=64)[:, :, 0:1], 0.0, [cmb])
            C0 = float(np.exp(-0.5))
            dsts = {n: Buf(n, dram=True) for n in ("Rs", "Ws", "Ks", "As", "Bs", "Gs", "BONs", "VFs", "Vtok")}
            ngr = cfg.NTOK // N
            bkc = [0]

            def nb():
                bkc[0] = (bkc[0] + 1) % 8
                return bkc[0]

            for g in range(ngr):
                s = g % 2
                n0 = g * N
                seq_start = (n0 % T) == 0
                self.ld(xc[s][:], X[:, :, n0:n0 + N].rearrange("c p n -> p c n"), xcb[s])
                if seq_start:
                    self.memset("dve", xp[s][:, :, 0:1], 0.0, [xpb[s]])
                    self.ld(xp[s][:, :, 1:N], X[:, :, n0:n0 + N - 1].rearrange("c p n -> p c n"), xpb[s])
                else:
                    self.ld(xp[s][:], X[:, :, n0 - 1:n0 + N - 1].rearrange("c p n -> p c n"), xpb[s])
                self.tt("dve", xx[:], xp[s][:], xc[s][:], ALU.subtract, [xpb[s], xcb[s]], [xxb])
                for c in range(6):
                    for k in range(KC):
                        self.stt(xm[c][:, k, :], xx[:, k, :], mixv(c)[:, k:k + 1], xc[s][:, k, :], ALU.mult, ALU.add,
                                 [xxb, xcb[s], vb], [xmb[c]])
                for (hid, w_, c, width, fn) in ((0, w1, 3, 64, AF.Tanh), (1, a1, 4, 64, AF.Identity),
                                                (2, g1, 5, 128, AF.Sigmoid), (3, v1, 2, 32, AF.Identity)):
                    if hid == 3 and j == 0:
                        continue
                    bk = nb()
                    for k in range(KC):
                        self.mm(self.ps[0:width, bk * 512:bk * 512 + N], w_[:, k, :], xm[c][:, k, :], k == 0, k == KC - 1,
                                [wsb, xmb[c]], [self.pb[bk]])
                    dstt = (hw, ha, hg, hv)[hid]
                    self.act(dstt[:], self.ps[0:width, bk * 512:bk * 512 + N], fn, [self.pb[bk]], [hb[hid]])
                for oc in range(KC):
                    o = oc % 2
                    osl = slice(oc * P, (oc + 1) * P)
                    for (c, name) in ((0, "r"), (1, "k"), (2, "v")):
                        bk = nb()
                        for k in range(KC):
                            self.mm(self.bkn(bk, N), wq[:, c, k, osl], xm[c][:, k, :], k == 0, k == KC - 1,
                                    [wqb, xmb[c]], [self.pb[bk]])
                        self.cp("act", tl[name][o][:], self.bkn(bk, N), [self.pb[bk]], [tb[name][o]])
                    bk = nb()
                    self.mm(self.bkn(bk, N), w2[:, osl], hw[:], True, True, [wsb, hb[0]], [self.pb[bk]])
                    self.act(tl["dec"][o][:], self.bkn(bk, N), AF.Sigmoid, [self.pb[bk], vb], [tb["dec"][o]],
                             bias=w0v[:, oc:oc + 1])
                    self.S.op("dve", lambda h, G_=tl["G"][o], d_=tl["dec"][o]: h.tensor_tensor_scan(
                        out=G_[:], data0=cmask[:], data1=d_[:], initial=0.0, op0=ALU.mult, op1=ALU.add),
                        [tb["dec"][o], cmb], [tb["G"][o]])
                    self.tt("dve", tl["gp"][o][:], tl["G"][o][:], tl["dec"][o][:], ALU.subtract,
                            [tb["G"][o], tb["dec"][o]], [tb["gp"][o]])
                    self.act(tl["gp"][o][:], tl["gp"][o][:], AF.Exp, [tb["gp"][o]], [tb["gp"][o]], scale=-C0)
                    self.act(tl["ig"][o][:], tl["G"][o][:], AF.Exp, [tb["G"][o]], [tb["ig"][o]], scale=C0)
                    self.act(tl["dec"][o][:], tl["G"][o][:], AF.Exp, [tb["G"][o]], [tb["dec"][o]], scale=-C0)
                    self.st(Ws[oc, :, n0:n0 + N], tl["dec"][o][:], dsts["Ws"], [tb["dec"][o]])
                    bk = nb()
                    self.mm(self.bkn(bk, N), a2[:, osl], ha[:], True, True, [wsb, hb[1]], [self.pb[bk]])
                    self.act(tl["a"][o][:], self.bkn(bk, N), AF.Sigmoid, [self.pb[bk], vb], [tb["a"][o]],
                             bias=a0v[:, oc:oc + 1])
                    bk = nb()
                    self.mm(self.bkn(bk, N), g2[:, osl], hg[:], True, True, [wsb, hb[2]], [self.pb[bk]])
                    self.cp("act", tl["g"][o][:], self.bkn(bk, N), [self.pb[bk]], [tb["g"][o]])
                    self.st(Gs[oc, :, n0:n0 + N], tl["g"][o][:], dsts["Gs"], [tb["g"][o]])
                    if j == 0:
                        self.st(VFs[oc, :, n0:n0 + N], tl["v"][o][:], dsts["VFs"], [tb["v"][o]])
                    else:
                        self.ld(tl["vf"][o][:], VFs[oc, :, n0:n0 + N], tb["vf"][o])
                        bk = nb()
                        self.mm(self.bkn(bk, N), v2[:, osl], hv[:], True, True, [wsb, hb[3]], [self.pb[bk]])
                        self.act(tl["t1"][o][:], self.bkn(bk, N), AF.Sigmoid, [self.pb[bk], vb], [tb["t1"][o]],
                                 bias=v0v[:, oc:oc + 1])
                        self.tt("dve", tl["vf"][o][:], tl["vf"][o][:], tl["v"][o][:], ALU.subtract,
                                [tb["vf"][o], tb["v"][o]], [tb["vf"][o]])
                        self.tt("dve", tl["vf"][o][:], tl["vf"][o][:], tl["t1"][o][:], ALU.mult,
                                [tb["vf"][o], tb["t1"][o]], [tb["vf"][o]])
                        self.tt("dve", tl["v"][o][:], tl["v"][o][:], tl["vf"][o][:], ALU.add,
                                [tb["vf"][o], tb["v"][o]], [tb["v"][o]])
                    self.cp("dve", vfm[s][:, oc, :], tl["v"][o][:], [tb["v"][o]], [vfmb[s]])
                    self.ts("dve", tl["kk"][o][:], tl["k"][o][:], kkv[:, oc:oc + 1], None, ALU.mult, None,
                            [tb["k"][o], vb], [tb["kk"][o]])
                    self.act(tl["t1"][o][:], tl["kk"][o][:], AF.Square, [tb["kk"][o]], [tb["t1"][o]])
                    bk = nb()
                    self.mm(self.bkn(bk, N), self.blk, tl["t1"][o][:], True, True, [tb["t1"][o], self.cbuf], [self.pb[bk]])
                    self.act(tl["t1"][o][:], self.bkn(bk, N), AF.Sqrt, [self.pb[bk]], [tb["t1"][o]])
                    self.ts("dve", tl["t1"][o][:], tl["t1"][o][:], 1e-12, None, ALU.max, None, [tb["t1"][o]], [tb["t1"][o]])
                    self.S.op("dve", lambda h, t=tl["t1"][o]: h.reciprocal(out=t[:], in_=t[:]), [tb["t1"][o]], [tb["t1"][o]])
                    self.tt("dve", tl["kk"][o][:], tl["kk"][o][:], tl["t1"][o][:], ALU.mult,
                            [tb["kk"][o], tb["t1"][o]], [tb["kk"][o]])
                    self.tt("dve", tl["t2"][o][:], tl["kk"][o][:], tl["a"][o][:], ALU.mult,
                            [tb["kk"][o], tb["a"][o]], [tb["t2"][o]])
                    self.tt("dve", tl["t2"][o][:], tl["t2"][o][:], tl["ig"][o][:], ALU.mult,
                            [tb["t2"][o], tb["ig"][o]], [tb["t2"][o]])
                    self.st(Bs[oc, :, n0:n0 + N], tl["t2"][o][:], dsts["Bs"], [tb["t2"][o]])
                    self.stt(tl["kk"][o][:], tl["kk"][o][:], -1.0, tl["gp"][o][:], ALU.mult, ALU.mult,
                             [tb["kk"][o], tb["gp"][o]], [tb["kk"][o]])
                    self.st(As[oc, :, n0:n0 + N], tl["kk"][o][:], dsts["As"], [tb["kk"][o]])
                    self.ts("dve", tl["a"][o][:], tl["a"][o][:], kav[:, oc:oc + 1], omka[:, oc:oc + 1], ALU.mult, ALU.add,
                            [tb["a"][o], vb], [tb["a"][o]])
                    self.tt("dve", tl["k"][o][:], tl["k"][o][:], tl["a"][o][:], ALU.mult, [tb["k"][o], tb["a"][o]], [tb["k"][o]])
                    self.stt(tl["t1"][o][:], tl["r"][o][:], rkv[:, oc:oc + 1], tl["k"][o][:], ALU.mult, ALU.mult,
                             [tb["r"][o], tb["k"][o], vb], [tb["t1"][o]])
                    bk = nb()
                    self.mm(self.bkn(bk, N), self.blk, tl["t1"][o][:], True, True, [tb["t1"][o], self.cbuf], [self.pb[bk]])
                    self.tt("dve", tl["t1"][o][:], self.bkn(bk, N), tl["v"][o][:], ALU.mult, [self.pb[bk], tb["v"][o]], [tb["t1"][o]])
                    self.st(BONs[oc, :, n0:n0 + N], tl["t1"][o][:], dsts["BONs"], [tb["t1"][o]])
                    self.tt("dve", tl["k"][o][:], tl["k"][o][:], tl["ig"][o][:], ALU.mult, [tb["k"][o], tb["ig"][o]], [tb["k"][o]])
                    self.st(Ks[oc, :, n0:n0 + N], tl["k"][o][:], dsts["Ks"], [tb["k"][o]])
                    self.tt("dve", tl["r"][o][:], tl["r"][o][:], tl["dec"][o][:], ALU.mult, [tb["r"][o], tb["dec"][o]], [tb["r"][o]])
                    self.st(Rs[oc, :, n0:n0 + N], tl["r"][o][:], dsts["Rs"], [tb["r"][o]])
                for a in range(NS):
                    for hf in range(2):
                        bk = nb()
                        pbf = self.bank(bk).bitcast(BF16)
                        for cc in range(4):
                            c = hf * 4 + cc
                            self.tr(pbf[:, cc * P:(cc + 1) * P], vfm[s][:, c, a * P:(a + 1) * P], self.identb[:],
                                    [vfmb[s], self.cbuf], [self.pb[bk]])
                        self.cp("act", vtb16[s][:, a, hf * 512:(hf + 1) * 512], pbf[:, 0:512], [self.pb[bk]], [vtb[s]])
                self.st(Vtok[n0:n0 + N, :].rearrange("(a p) d -> p a d", p=P), vtb16[s][:], dsts["Vtok"], [vtb[s]])
            self.end_phase("rwkv_proj")

    def phase_scan(self, Rs, Ws, Ks, As, Bs, Vtok, Ys):
        cfg, nc = self.cfg, self.nc
        T, NB = cfg.T, cfg.NB
        TC = 64
        YG = 16
        with ExitStack() as st:
            sb = lambda name, shape, dt=F32: st.enter_context(nc.sbuf_tensor(self.un(name), shape, dt))
            par = {n: [sb(f"sc_{n}{i}", [P, NB * KC, TC]) for i in range(2)] for n in ("r", "w", "k", "a", "b")}
            parb = {n: [Buf(f"{n}0"), Buf(f"{n}1")] for n in par}
            src = dict(r=Rs, w=Ws, k=Ks, a=As, b=Bs)
            vt = [sb(f"sc_vt{i}", [P, NB, D], BF16) for i in range(2)]; vtb = [Buf("vt0"), Buf("vt1")]
            Sb = [self.pb[b] for b in range(NB)]
            Xa = [sb(f"sc_Xa{b}", [P, 512], BF16) for b in range(NB)]; Xab = [Buf(f"Xa{b}") for b in range(NB)]
            Xr = [sb(f"sc_Xr{b}", [P, 512], BF16) for b in range(NB)]; Xrb = [Buf(f"Xr{b}") for b in range(NB)]
            T2 = [sb(f"sc_T2{b}", [P, 512], BF16) for b in range(NB)]; T2b = [Buf(f"T2{b}") for b in range(NB)]
            KV = [sb(f"sc_KV{b}", [P, 512], BF16) for b in range(NB)]; KVb = [Buf(f"KV{b}") for b in range(NB)]
            rsc = [sb(f"sc_rsc{i}", [P, 512]) for i in range(2)]; rscb = [Buf("rsc0"), Buf("rsc1")]
            zero = sb("sc_zero", [P, 512], BF16); zb = Buf("zero")
            sel = sb("sc_sel", [P, P, HS], BF16); selb = Buf("sel")
            yst = [sb(f"sc_y{i}", [P, NB * 4 * 2, YG]) for i in range(2)]; ystb = [Buf("y0"), Buf("y1")]
            ydst = Buf("Ys", dram=True)
            self.cp("dve", sel[:], self.identb[:].unsqueeze(2).to_broadcast([P, P, HS]), [selb, self.cbuf], [selb])
            self.memset("dve", zero[:], 0.0, [zb])
            for b in range(NB):
                self.mm(self.bank(b), self.identb[:], zero[:], True, True, [zb, self.cbuf], [Sb[b]])
            v3p = lambda bk: self.bank(bk).rearrange("p (c i) -> p c i", i=HS)
            v3 = lambda tile: tile[:].rearrange("p (c i) -> p c i", i=HS)
            VBK, YBK = 6, 7
            for t in range(T):
                ci, tl = divmod(t, TC)
                cs = ci % 2
                if tl == 0:
                    for n in par:
                        for b in range(NB):
                            self.ld(par[n][cs][:, b * KC:(b + 1) * KC, :],
                                    src[n][:, :, b * T + t: b * T + t + TC].rearrange("c p n -> p c n"), parb[n][cs])
                vi, tv = divmod(t, P)
                vs = vi % 2
                if tv == 0:
                    for b in range(NB):
                        self.ld(vt[vs][:, b, :], Vtok[b * T + t: b * T + t + P, :], vtb[vs])
                yi, ty = divmod(t, YG)
                bc = lambda n, b: par[n][cs][:, b * KC:(b + 1) * KC, tl].unsqueeze(2).to_broadcast([P, KC, HS])
                for b in range(NB):
                    self.tt("dve", v3(Xa[b]), v3p(b), bc("a", b), ALU.mult, [Sb[b], parb["a"][cs]], [Xab[b]])
                for b in range(NB):
                    sab = 4 + (b % 2)
                    self.mm(self.bank(sab), self.blkb[:], Xa[b][:], True, True, [Xab[b], self.cbuf], [self.pb[sab]])
                    for pl in range(2):
                        rhs = vt[vs][:, b, :].rearrange("p (c l i) -> p c l i", l=2, i=HS)[:, :, pl, :]
                        self.mm(self.ps[pl * HS:(pl + 1) * HS, VBK * 512:(VBK + 1) * 512], sel[:, tv, :], rhs, True, True,
                                [vtb[vs], selb], [self.pb[VBK]])
                    self.tt("dve", v3(KV[b]), v3p(VBK), bc("k", b), ALU.mult, [self.pb[VBK], parb["k"][cs]], [KVb[b]])
                    self.tt("dve", v3(T2[b]), v3p(sab), bc("b", b), ALU.mult, [self.pb[sab], parb["b"][cs]], [T2b[b]])
                    self.S.op("pe", lambda h, o=self.bank(b), r_=T2[b][:]: h.matmul(o, self.identb[:], r_, start=False, stop=True,
                                                                                     skip_group_check=True),
                              [T2b[b], self.cbuf], [Sb[b]])
                    self.S.op("pe", lambda h, o=self.bank(b), r_=KV[b][:]: h.matmul(o, self.identb[:], r_, start=False, stop=True,
                                                                                     skip_group_check=True),
                              [KVb[b], self.cbuf], [Sb[b]])
                for b in range(NB):
                    self.tt("dve", v3(Xr[b]), v3p(b), bc("r", b), ALU.mult, [Sb[b], parb["r"][cs]], [Xrb[b]])
                    for q in range(4):
                        col = YBK * 512 + ty * (NB * 8) + b * 8 + q * 2
                        self.mm(self.ps[:, col:col + 2], Xr[b][:, q * P:(q + 1) * P], self.blk2b[:], True, True,
                                [Xrb[b], self.cbuf], [self.pb[YBK]])
                if ty == YG - 1:
                    ys = yi % 2
                    self.cp("act", yst[ys][:].rearrange("p c a -> p a c"),
                            self.ps[:, YBK * 512: YBK * 512 + YG * NB * 8].rearrange("p (a c) -> p a c", a=YG),
                            [self.pb[YBK]], [ystb[ys]])
                    t0 = t - YG + 1
                    for b in range(NB):
                        self.st(Ys[:, :, b * T + t0: b * T + t0 + YG].rearrange("c p n -> p c n"),
                                yst[ys][:, b * 8:(b + 1) * 8, :], ydst, [ystb[ys]], q="pool")
                if tl == TC - 1 and t != T - 1:
                    for b in range(NB):
                        rs_ = b % 2
                        self.tt("dve", v3(rsc[rs_]), v3p(b), bc("w", b), ALU.mult, [Sb[b], parb["w"][cs]], [rscb[rs_]])
                        self.mm(self.bank(b), self.ident, rsc[rs_][:], True, True, [rscb[rs_], self.cbuf], [Sb[b]])
            self.end_phase("scan")

    def perm_src(self, A, n0, N, u):
        v = A[:, :, n0:n0 + N].rearrange("(q u) (l i) n -> u i q l n", u=2, l=2)
        return v[u]

    def phase_rwkv_out(self, X, Xn, j, li, vr_in, vl_in, wo_in, Ys, Gs, BONs):
        cfg, nc = self.cfg, self.nc
        N = 256
        with ExitStack() as st:
            sb = lambda name, shape, dt=F32: st.enter_context(nc.sbuf_tensor(self.un(name), shape, dt))
            vec = sb("ro_vec", [P, 15 * 8]); vb = Buf("vec")
            self.ld(vec[:], vr_in[j], vb)
            lg = vec[:, 48 + 6 * 8:48 + 7 * 8]; lb = vec[:, 48 + 7 * 8:48 + 8 * 8]
            vl = sb("ro_vl", [P, 7 * 8])
            self.ld(vl[:], vl_in[li], vb)
            wo = sb("ro_wo", [P, KC, D], BF16); wob = Buf("wo")
            wv = wo_in[j].rearrange("(q u l i) o -> u i q l o", q=4, u=2, l=2)
            for u in range(2):
                for q in range(4):
                    self.wload(wo[u * HS:(u + 1) * HS, q * 2:(q + 1) * 2, :], wv[u][:, q], wob)
            y = [sb(f"ro_y{i}", [P, KC, N]) for i in range(2)]; yb = [Buf("y0"), Buf("y1")]
            gg = [sb(f"ro_g{i}", [P, KC, N]) for i in range(2)]; ggb = [Buf("g0"), Buf("g1")]
            bo = [sb(f"ro_b{i}", [P, KC, N]) for i in range(2)]; bob = [Buf("b0"), Buf("b1")]
            xr = [sb(f"ro_x{i}", [P, KC, N]) for i in range(2)]; xrb = [Buf("x0"), Buf("x1")]
            sq = sb("ro_sq", [P, N]); sqb = Buf("sq")
            mean = sb("ro_mean", [P, N]); meanb = Buf("mean")
            rs = sb("ro_rs", [P, N]); rsb = Buf("rs")
            yg = sb("ro_yg", [P, KC, N], BF16); ygb = Buf("yg")
            tmp, tmpb = self.ln_tmp(st, N, "ro")
            dst = Buf("Xn", dram=True)
            bkc = [0]

            def nb():
                bkc[0] = (bkc[0] + 1) % 6
                return bkc[0]

            for g in range(cfg.NTOK // N):
                s = g % 2
                n0 = g * N
                self.ld(y[s][:], Ys[:, :, n0:n0 + N].rearrange("c p n -> p c n"), yb[s])
                for u in range(2):
                    for q in range(4):
                        self.ld(gg[s][u * HS:(u + 1) * HS, q * 2:(q + 1) * 2, :], self.perm_src(Gs, n0, N, u)[:, q], ggb[s])
                        self.ld(bo[s][u * HS:(u + 1) * HS, q * 2:(q + 1) * 2, :], self.perm_src(BONs, n0, N, u)[:, q], bob[s])
                self.ld(xr[s][:], X[:, :, n0:n0 + N].rearrange("c p n -> p c n"), xrb[s])
                for c in range(KC):
                    bm, bq = nb(), nb()
                    self.act(sq[:], y[s][:, c, :], AF.Square, [yb[s]], [sqb])
                    self.mm(self.bkn(bm, N), self.blk64, y[s][:, c, :], True, True, [yb[s], self.cbuf], [self.pb[bm]])
                    self.mm(self.bkn(bq, N), self.blk64, sq[:], True, True, [sqb, self.cbuf], [self.pb[bq]])
                    self.cp("act", mean[:], self.bkn(bm, N), [self.pb[bm]], [meanb])
                    self.tt("dve", rs[:], mean[:], mean[:], ALU.mult, [meanb], [rsb])
                    self.tt("dve", rs[:], self.bkn(bq, N), rs[:], ALU.subtract, [self.pb[bq], rsb], [rsb])
                    self.ts("dve", rs[:], rs[:], 0.0, GN_EPS, ALU.max, ALU.add, [rsb], [rsb])
                    self.act(rs[:], rs[:], AF.Sqrt, [rsb], [rsb])
                    self.S.op("dve", lambda h: h.reciprocal(out=rs[:], in_=rs[:]), [rsb], [rsb])
                    self.tt("dve", y[s][:, c, :], y[s][:, c, :], mean[:], ALU.subtract, [yb[s], meanb], [yb[s]])
                    self.tt("dve", y[s][:, c, :], y[s][:, c, :], rs[:], ALU.mult, [yb[s], rsb], [yb[s]])
                    self.act(y[s][:, c, :], y[s][:, c, :], AF.Identity, [yb[s], vb], [yb[s]],
                             bias=lb[:, c:c + 1], scale=lg[:, c:c + 1])
                    self.tt("dve", y[s][:, c, :], y[s][:, c, :], bo[s][:, c, :], ALU.add, [yb[s], bob[s]], [yb[s]])
                    self.tt("dve", yg[:, c, :], y[s][:, c, :], gg[s][:, c, :], ALU.mult, [yb[s], ggb[s]], [ygb])
                for oc in range(KC):
                    bk = nb()
                    for k in range(KC):
                        self.mm(self.bkn(bk, N), wo[:, k, oc * P:(oc + 1) * P], yg[:, k, :], k == 0, k == KC - 1,
                                [wob, ygb], [self.pb[bk]])
                    self.stt(xr[s][:, oc, :], xr[s][:, oc, :], cfg.alpha, self.bkn(bk, N), ALU.mult, ALU.add,
                             [xrb[s], self.pb[bk]], [xrb[s]])
                self.ln_fm(xr[s], xrb[s], N, vl[:, 0:8], vl[:, 8:16], tmp, tmpb, 6, vb)
                self.st(Xn[:, :, n0:n0 + N].rearrange("c p n -> p c n"), xr[s][:], dst, [xrb[s]])
            self.end_phase("rwkv_out")

    def phase_conv(self, X, Xn, j, li, vc_in, vl_in, cwin_in, cwout_in):
        cfg, nc = self.cfg, self.nc
        N = 512
        T = cfg.T
        with ExitStack() as st:
            sb = lambda name, shape, dt=F32: st.enter_context(nc.sbuf_tensor(self.un(name), shape, dt))
            vc = sb("cv_vc", [P, 24]); vb = Buf("vec")
            self.ld(vc[:], vc_in[j], vb)
            vl = sb("cv_vl", [P, 56]); self.ld(vl[:], vl_in[li], vb)
            win = sb("cv_win", [P, KC, 3 * D], BF16); winb = Buf("win")
            for k in range(KC):
                self.wload(win[:, k, :], cwin_in[j, k * P:(k + 1) * P, :], winb)
            wout = sb("cv_wout", [P, KC, D], BF16); woutb = Buf("wout")
            self.wload(wout[:], cwout_in[j].rearrange("(k p) o -> p k o", p=P), woutb)
            xr = [sb(f"cv_x{i}", [P, KC, N]) for i in range(2)]; xrb = [Buf("x0"), Buf("x1")]
            xbf = sb("cv_xbf", [P, KC, N], BF16); xbfb = Buf("xbf")
            gb = sb("cv_gb", [P, KC, N], BF16); gbb = Buf("gb")
            gc = sb("cv_gc", [P, N]); gcb = Buf("gc")
            ch = sb("cv_ch", [P, KC, N + 2]); chb = Buf("ch")
            u = sb("cv_u", [P, N]); ub = Buf("u")
            z = sb("cv_z", [P, KC, N], BF16); zb = Buf("z")
            tmp, tmpb = self.ln_tmp(st, N, "cv")
            dst = Buf("Xn", dram=True)
            bkc = [0]

            def nb():
                bkc[0] = (bkc[0] + 1) % 6
                return bkc[0]

            for g in range(cfg.NTOK // N):
                s = g % 2
                n0 = g * N
                self.ld(xr[s][:], X[:, :, n0:n0 + N].rearrange("c p n -> p c n"), xrb[s])
                self.cp("act", xbf[:], xr[s][:], [xrb[s]], [xbfb])
                if (n0 % T) == 0:
                    self.memset("dve", ch[:, :, 0:2], 0.0, [chb])
                else:
                    self.cp("dve", ch[:, :, 0:2], ch[:, :, N:N + 2], [chb], [chb])
                for oc in range(KC):
                    bk = nb()
                    for k in range(KC):
                        self.mm(self.bank(bk), win[:, k, oc * P:(oc + 1) * P], xbf[:, k, :], k == 0, k == KC - 1,
                                [winb, xbfb], [self.pb[bk]])
                    self.cp("act", gb[:, oc, :], self.bank(bk), [self.pb[bk]], [gbb])
                    bk = nb()
                    for k in range(KC):
                        self.mm(self.bank(bk), win[:, k, D + oc * P:D + (oc + 1) * P], xbf[:, k, :], k == 0, k == KC - 1,
                                [winb, xbfb], [self.pb[bk]])
                    self.cp("act", gc[:], self.bank(bk), [self.pb[bk]], [gcb])
                    bk = nb()
                    for k in range(KC):
                        self.mm(self.bank(bk), win[:, k, 2 * D + oc * P:2 * D + (oc + 1) * P], xbf[:, k, :], k == 0, k == KC - 1,
                                [winb, xbfb], [self.pb[bk]])
                    self.tt("dve", ch[:, oc, 2:N + 2], self.bank(bk), gc[:], ALU.mult, [self.pb[bk], gcb], [chb])
                    self.ts("dve", u[:], ch[:, oc, 2:N + 2], vc[:, 16 + oc:17 + oc], None, ALU.mult, None, [chb, vb], [ub])
                    self.stt(u[:], ch[:, oc, 1:N + 1], vc[:, 8 + oc:9 + oc], u[:], ALU.mult, ALU.add, [chb, ub, vb], [ub])
                    self.stt(u[:], ch[:, oc, 0:N], vc[:, oc:oc + 1], u[:], ALU.mult, ALU.add, [chb, ub, vb], [ub])
                    self.tt("dve", z[:, oc, :], u[:], gb[:, oc, :], ALU.mult, [ub, gbb], [zb])
                for oc in range(KC):
                    bk = nb()
                    for k in range(KC):
                        self.mm(self.bank(bk), wout[:, k, oc * P:(oc + 1) * P], z[:, k, :], k == 0, k == KC - 1,
                                [woutb, zb], [self.pb[bk]])
                    self.stt(xr[s][:, oc, :], xr[s][:, oc, :], cfg.alpha, self.bank(bk), ALU.mult, ALU.add,
                             [xrb[s], self.pb[bk]], [xrb[s]])
                self.ln_fm(xr[s], xrb[s], N, vl[:, 0:8], vl[:, 8:16], tmp, tmpb, 6, vb)
                self.st(Xn[:, :, n0:n0 + N].rearrange("c p n -> p c n"), xr[s][:], dst, [xrb[s]])
            self.end_phase("conv")

    def phase_moe(self, X, li, rw_in, rb_in, wgu_in, wdn_in, bgu_in, bdn_in, FFN):
        cfg, nc = self.cfg, self.nc
        E = cfg.E
        NG = 1024 if cfg.NTOK % 1024 == 0 else 512
        NT = NG // P
        with ExitStack() as st:
            sb = lambda name, shape, dt=F32: st.enter_context(nc.sbuf_tensor(self.un(name), shape, dt))
            rw = sb("mo_rw", [P, KC, E]); cb = Buf("const")
            self.ld(rw[:], rw_in[li].rearrange("(k p) e -> p k e", p=P), cb)
            rbias = sb("mo_rb", [1, E]); self.ld(rbias[:], rb_in[li], cb)
            ones1 = sb("mo_ones", [1, P]); self.memset("dve", ones1[:], 1.0, [cb])
            bgu = sb("mo_bgu", [P, E * 16]); self.ld(bgu[:], bgu_in[li], cb)
            bdn = sb("mo_bdn", [E, D]); self.ld(bdn[:], bdn_in[li], cb)
            big = sb("mo_big", [P, NT * D]); bigb = Buf("big")
            xbf = sb("mo_xbf", [P, KC, NG], BF16); xbfb = Buf("xbf")
            gate = sb("mo_gate", [P, NT, E]); gateb = Buf("gate")
            gT = sb("mo_gT", [E, NG]); gTb = Buf("gT")
            lg = sb("mo_lg", [P, E]); lgb = Buf("lg")
            m8 = sb("mo_m8", [P, 8]); nmx = sb("mo_nmx", [P, 1]); msk = sb("mo_msk", [P, E]); ssum = sb("mo_ssum", [P, 1])
            smb = Buf("small")
            wgu = [sb(f"mo_wgu{i}", [P, KC, 2 * D], BF16) for i in range(2)]; wgub = [Buf("wgu0"), Buf("wgu1")]
            wdn = sb("mo_wdn", [P, KC, D], BF16); wdnb = Buf("wdn")
            actt = sb("mo_act", [P, KC, 512], BF16); actb = [Buf(f"act{c}") for c in range(KC)]
            tg = [sb(f"mo_tg{i}", [P, 512]) for i in range(2)]; tgb = [Buf("tg0"), Buf("tg1")]
            tsg = [sb(f"mo_ts{i}", [P, 512]) for i in range(2)]; tsb = [Buf("ts0"), Buf("ts1")]
            tln = [sb(f"mo_tl{i}", [P, 512]) for i in range(2)]; tlb = [Buf("tl0"), Buf("tl1")]
            dst = Buf("FFN", dram=True)
            xv = big[:].rearrange("p (c n) -> p c n", c=KC)
            acc = big[:].rearrange("p (t d) -> p t d", t=NT)
            bkc = [0]

            def nb():
                bkc[0] = (bkc[0] + 1) % 8
                return bkc[0]

            ngroups = cfg.NTOK // NG
            ecount = 0
            for g in range(ngroups):
                n0 = g * NG
                self.ld(xv, X[:, :, n0:n0 + NG].rearrange("c p n -> p c n"), bigb)
                self.cp("act", xbf[:], xv, [bigb], [xbfb])
                for t in range(NT):
                    bk = nb()
                    for k in range(KC):
                        self.mm(self.ps[:, bk * 512:bk * 512 + E], xv[:, k, t * P:(t + 1) * P], rw[:, k, :], k == 0, False,
                                [bigb, cb], [self.pb[bk]])
                    self.mm(self.ps[:, bk * 512:bk * 512 + E], ones1[:], rbias[:], False, True, [cb], [self.pb[bk]])
                    self.cp("dve", lg[:], self.ps[:, bk * 512:bk * 512 + E], [self.pb[bk]], [lgb])
                    self.S.op("dve", lambda h: h.max(out=m8[:], in_=lg[:]), [lgb], [smb])
                    self.ts("dve", msk[:], lg[:], m8[:, TOPK - 1:TOPK], None, ALU.is_ge, None, [lgb, smb], [smb])
                    self.ts("dve", nmx[:], m8[:, 0:1], -1.0, None, ALU.mult, None, [smb], [smb])
                    self.act(lg[:], lg[:], AF.Exp, [lgb, smb], [lgb], bias=nmx[:, 0:1])
                    self.tt("dve", lg[:], lg[:], msk[:], ALU.mult, [lgb, smb], [lgb])
                    self.S.op("dve", lambda h: h.reduce_sum(out=ssum[:], in_=lg[:], axis=mybir.AxisListType.X), [lgb], [smb])
                    self.S.op("dve", lambda h: h.reciprocal(out=ssum[:], in_=ssum[:]), [smb], [smb])
                    self.ts("dve", gate[:, t, :], lg[:], ssum[:, 0:1], None, ALU.mult, None, [lgb, smb], [gateb])
                    bk = nb()
                    self.tr(self.ps[0:E, bk * 512:bk * 512 + P], gate[:, t, :], self.ident, [gateb, self.cbuf], [self.pb[bk]])
                    self.cp("act", gT[:, t * P:(t + 1) * P], self.ps[0:E, bk * 512:bk * 512 + P], [self.pb[bk]], [gTb])
                if getattr(cfg, "debug", None) == "gate":
                    self.S.dma("sp", self.y_out[n0:n0 + NG, 0:E].rearrange("(t p) e -> p t e", p=P), gate[:], [gateb], [dst])
                    self.S.dma("sp", self.y_out[n0:n0 + P, 64:64 + 8], m8[:], [smb], [dst])
                    self.S.dma("sp", self.y_out[n0:n0 + P, 128:128 + E], msk[:], [smb], [dst])
                    self.S.dma("sp", self.y_out[n0:n0 + P, 192:192 + 8], m8[:], [smb], [dst])
                    self.S.dma("sp", self.y_out[n0:n0 + P, 256:256 + E], lg[:], [lgb], [dst])
                for t in range(NT):
                    for hf in range(2):
                        bk = nb()
                        self.mm(self.bank(bk), gT[:, t * P:(t + 1) * P], bdn[:, hf * 512:(hf + 1) * 512], True, True,
                                [gTb, cb], [self.pb[bk]])
                        self.cp("act", acc[:, t, hf * 512:(hf + 1) * 512], self.bank(bk), [self.pb[bk]], [bigb])
                for e in range(E):
                    ws = ecount % 2
                    ecount += 1
                    for k in range(KC):
                        self.wload(wgu[ws][:, k, :], wgu_in[li, e, k * P:(k + 1) * P, :], wgub[ws])
                    self.wload(wdn[:], wdn_in[li, e].rearrange("(k p) o -> p k o", p=P), wdnb)
                    for sub in range(NG // 512):
                        tok = slice(sub * 512, (sub + 1) * 512)
                        for fc in range(KC):
                            o = fc % 2
                            bkg = nb()
                            for k in range(KC):
                                self.mm(self.bank(bkg), wgu[ws][:, k, fc * P:(fc + 1) * P], xbf[:, k, tok], k == 0, k == KC - 1,
                                        [wgub[ws], xbfb], [self.pb[bkg]])
                            bkl = nb()
                            for k in range(KC):
                                self.mm(self.bank(bkl), wgu[ws][:, k, D + fc * P:D + (fc + 1) * P], xbf[:, k, tok], k == 0,
                                        k == KC - 1, [wgub[ws], xbfb], [self.pb[bkl]])
                            bg_ = bgu[:, e * 16 + fc:e * 16 + fc + 1]
                            bl_ = bgu[:, e * 16 + 8 + fc:e * 16 + 8 + fc + 1]
                            self.ts("dve", tg[o][:], self.bank(bkg), bg_, 7.0, ALU.add, ALU.min, [self.pb[bkg], cb], [tgb[o]])
                            self.act(tsg[o][:], tg[o][:], AF.Sigmoid, [tgb[o]], [tsb[o]], scale=1.702)
                            self.ts("dve", tln[o][:], self.bank(bkl), bl_, 7.0, ALU.add, ALU.min, [self.pb[bkl], cb], [tlb[o]])
                            self.ts("dve", tln[o][:], tln[o][:], -7.0, 1.0, ALU.max, ALU.add, [tlb[o]], [tlb[o]])
                            self.tt("dve", tg[o][:], tg[o][:], tsg[o][:], ALU.mult, [tgb[o], tsb[o]], [tgb[o]])
                            self.tt("dve", actt[:, fc, :], tg[o][:], tln[o][:], ALU.mult, [tgb[o], tlb[o]], [actb[fc]])
                        for tt_ in range(4):
                            t = sub * 4 + tt_
                            for hf in range(2):
                                bk = nb()
                                for k in range(KC):
                                    self.mm(self.bank(bk), actt[:, k, tt_ * P:(tt_ + 1) * P], wdn[:, k, hf * 512:(hf + 1) * 512],
                                            k == 0, k == KC - 1, [actb[k], wdnb], [self.pb[bk]])
                                self.stt(acc[:, t, hf * 512:(hf + 1) * 512], self.bank(bk), gate[:, t, e:e + 1],
                                         acc[:, t, hf * 512:(hf + 1) * 512], ALU.mult, ALU.add,
                                         [self.pb[bk], gateb, bigb], [bigb])
                self.st(FFN[n0:n0 + NG, :].rearrange("(t p) d -> p t d", p=P), acc, dst, [bigb])
            self.end_phase("moe")

    def phase_moe_epi(self, X, Xn, li, vl_in, FFN):
        cfg, nc = self.cfg, self.nc
        N = 512
        with ExitStack() as st:
            sb = lambda name, shape, dt=F32: st.enter_context(nc.sbuf_tensor(self.un(name), shape, dt))
            vl = sb("me_vl", [P, 56]); vb = Buf("vec"); self.ld(vl[:], vl_in[li], vb)
            xr = [sb(f"me_x{i}", [P, KC, N]) for i in range(2)]; xrb = [Buf("x0"), Buf("x1")]
            ft = [sb(f"me_f{i}", [P, 4, D]) for i in range(2)]; ftb = [Buf("f0"), Buf("f1")]
            tmp, tmpb = self.ln_tmp(st, N, "me")
            dst = Buf("Xn", dram=True)
            for g in range(cfg.NTOK // N):
                s = g % 2
                n0 = g * N
                self.ld(xr[s][:], X[:, :, n0:n0 + N].rearrange("c p n -> p c n"), xrb[s])
                self.ld(ft[s][:], FFN[n0:n0 + N, :].rearrange("(a p) d -> p a d", p=P), ftb[s])
                for c in range(KC):
                    bk = (g * KC + c) % 6
                    for a in range(4):
                        self.tr(self.ps[:, bk * 512 + a * P: bk * 512 + (a + 1) * P], ft[s][:, a, c * P:(c + 1) * P],
                                self.ident, [ftb[s], self.cbuf], [self.pb[bk]])
                    self.stt(xr[s][:, c, :], xr[s][:, c, :], cfg.alpha, self.bank(bk), ALU.mult, ALU.add,
                             [xrb[s], self.pb[bk]], [xrb[s]])
                self.ln_fm(xr[s], xrb[s], N, vl[:, 16:24], vl[:, 24:32], tmp, tmpb, 6, vb)
                self.st(Xn[:, :, n0:n0 + N].rearrange("c p n -> p c n"), xr[s][:], dst, [xrb[s]])
            self.end_phase("moe_epi")

    def phase_ple(self, X, Xn, li, vl_in, p_in, pproj_in, pgate_in):
        cfg, nc = self.cfg, self.nc
        N = 512
        with ExitStack() as st:
            sb = lambda name, shape, dt=F32: st.enter_context(nc.sbuf_tensor(self.un(name), shape, dt))
            vl = sb("pl_vl", [P, 56]); vb = Buf("vec"); self.ld(vl[:], vl_in[li], vb)
            wp = sb("pl_wp", [P, 2, D], BF16); wg = sb("pl_wg", [P, KC, D], BF16); wb = Buf("w")
            self.wload(wp[:], pproj_in[li].rearrange("(k p) o -> p k o", p=P), wb)
            self.wload(wg[:], pgate_in[li].rearrange("(k p) o -> p k o", p=P), wb)
            xr = [sb(f"pl_x{i}", [P, KC, N]) for i in range(2)]; xrb = [Buf("x0"), Buf("x1")]
            xbf = sb("pl_xbf", [P, KC, N], BF16); xbfb = Buf("xbf")
            pt = [sb(f"pl_p{i}", [P, 4, PLE]) for i in range(2)]; ptb = [Buf("p0"), Buf("p1")]
            pf = sb("pl_pf", [P, 2, N], BF16); pfb = Buf("pf")
            sg = sb("pl_sg", [P, N]); sgb = Buf("sg")
            tmp, tmpb = self.ln_tmp(st, N, "pl")
            dst = Buf("Xn", dram=True)
            bkc = [0]

            def nb():
                bkc[0] = (bkc[0] + 1) % 6
                return bkc[0]

            for g in range(cfg.NTOK // N):
                s = g % 2
                n0 = g * N
                self.ld(xr[s][:], X[:, :, n0:n0 + N].rearrange("c p n -> p c n"), xrb[s])
                self.ld(pt[s][:], p_in[li, n0:n0 + N, :].rearrange("(a p) d -> p a d", p=P), ptb[s])
                self.cp("act", xbf[:], xr[s][:], [xrb[s]], [xbfb])
                for c in range(2):
                    bk = nb()
                    for a in range(4):
                        self.tr(self.ps[:, bk * 512 + a * P: bk * 512 + (a + 1) * P], pt[s][:, a, c * P:(c + 1) * P],
                                self.ident, [ptb[s], self.cbuf], [self.pb[bk]])
                    self.cp("act", pf[:, c, :], self.bank(bk), [self.pb[bk]], [pfb])
                for oc in range(KC):
                    bk = nb()
                    for k in range(KC):
                        self.mm(self.bank(bk), wg[:, k, oc * P:(oc + 1) * P], xbf[:, k, :], k == 0, k == KC - 1,
                                [wb, xbfb], [self.pb[bk]])
                    self.act(sg[:], self.bank(bk), AF.Sigmoid, [self.pb[bk], vb], [sgb], bias=vl[:, 32 + oc:33 + oc])
                    bk = nb()
                    for k in range(2):
                        self.mm(self.bank(bk), wp[:, k, oc * P:(oc + 1) * P], pf[:, k, :], k == 0, k == 1,
                                [wb, pfb], [self.pb[bk]])
                    self.tt("dve", sg[:], self.bank(bk), sg[:], ALU.mult, [self.pb[bk], sgb], [sgb])
                    self.stt(xr[s][:, oc, :], xr[s][:, oc, :], cfg.alpha, sg[:], ALU.mult, ALU.add, [xrb[s], sgb], [xrb[s]])
                self.ln_fm(xr[s], xrb[s], N, vl[:, 40:48], vl[:, 48:56], tmp, tmpb, 6, vb)
                self.st(Xn[:, :, n0:n0 + N].rearrange("c p n -> p c n"), xr[s][:], dst, [xrb[s]])
            self.end_phase("ple")


def _fm(v):
    v = np.asarray(v, np.float32)
    lead = int(np.prod(v.shape[:-1])) if v.ndim > 1 else 1
    return np.ascontiguousarray(v.reshape(lead, KC, P).transpose(2, 0, 1).reshape(P, lead * KC))


def _fm_perm(v):
    v = np.asarray(v, np.float32).reshape(4, 2, 2, HS)
    return np.ascontiguousarray(v.transpose(1, 3, 0, 2).reshape(P, 8))


def make_consts():
    ident = np.eye(P, dtype=np.float32)
    onesD = np.full((P, P), 1.0 / D, np.float32)
    blk = np.zeros((P, P), np.float32)
    blk[:HS, :HS] = 1.0
    blk[HS:, HS:] = 1.0
    cst = np.concatenate([ident, onesD, blk, blk / HS], axis=1)
    blk2 = np.zeros((P, 2), np.float32)
    blk2[:HS, 0] = 1.0
    blk2[HS:, 1] = 1.0
    return np.ascontiguousarray(cst), blk2


def prep_shared(cfg, inp):
    L, E, NR, NCV = cfg.L, cfg.E, cfg.NR, cfg.NCV
    f = lambda k: np.asarray(inp[k], np.float32)
    cst, blk2 = make_consts()
    vr = np.zeros((NR, P, 15 * 8), np.float32)
    for j in range(NR):
        cols = [_fm(f("rwkv_mix")[j])]
        cols.append(_fm(f("rwkv_w0")[j])); cols.append(_fm(f("rwkv_a0")[j]))
        cols.append(_fm(f("rwkv_v0")[j - 1]) if j > 0 else np.zeros((P, 8), np.float32))
        cols.append(_fm(f("rwkv_k_k")[j])); cols.append(_fm(f("rwkv_k_a")[j]))
        cols.append(_fm(f("rwkv_r_k")[j].reshape(D)))
        cols.append(_fm_perm(f("rwkv_lnx_g")[j])); cols.append(_fm_perm(f("rwkv_lnx_b")[j]))
        cols.append(np.zeros((P, 8), np.float32))
        vr[j] = np.concatenate(cols, axis=1)
    vc = np.zeros((max(NCV, 1), P, 24), np.float32)
    for j in range(NCV):
        vc[j] = _fm(f("conv_w")[j])
    vl = np.zeros((L, P, 56), np.float32)
    for i in range(L):
        vl[i] = np.concatenate([_fm(f(k)[i]) for k in ("ln_mix_g", "ln_mix_b", "ln_ffn_g", "ln_ffn_b", "ple_b_gate",
                                                       "ln_ple_g", "ln_ple_b")], axis=1)
    bgu = np.stack([np.ascontiguousarray(f("moe_b_gu")[i].reshape(E, 16, P).transpose(2, 0, 1).reshape(P, E * 16))
                    for i in range(L)])
    d = dict(cst=cst, blk2=blk2, vec_rwkv=vr, vec_conv=vc, vec_layer=vl, b_gu=bgu,
             b_down=f("moe_b_down"), router_b=f("router_b").reshape(L, 1, E), router_w=f("router_w"),
             w_rkv=f("rwkv_w_rkv"), w1=f("rwkv_w1"), w2=f("rwkv_w2"), a1=f("rwkv_a1"), a2=f("rwkv_a2"),
             g1=f("rwkv_g1"), g2=f("rwkv_g2"), w_o=f("rwkv_w_o"),
             conv_w_in=f("conv_w_in"), conv_w_out=f("conv_w_out"),
             moe_w_gu=f("moe_w_gu"), moe_w_down=f("moe_w_down"),
             ple_w_proj=f("ple_w_proj"), ple_w_gate=f("ple_w_gate"))
    if NR > 1:
        d["v1"] = f("rwkv_v1"); d["v2"] = f("rwkv_v2")
    else:
        d["v1"] = np.zeros((1, D, 32), np.float32); d["v2"] = np.zeros((1, 32, D), np.float32)
    if NCV == 0:
        d["conv_w_in"] = np.zeros((1, D, 3 * D), np.float32); d["conv_w_out"] = np.zeros((1, D, D), np.float32)
    return d


_NC_CACHE = {}


def run(cfg, inp, n_cores):
    key = (cfg.T, cfg.NB, cfg.E, cfg.L, getattr(cfg, 'stop', None), getattr(cfg, 'debug', None))
    if key not in _NC_CACHE:
        _NC_CACHE[key] = Builder(cfg).build()
    nc = _NC_CACHE[key]
    shared = prep_shared(cfg, inp)
    x = np.asarray(inp["x"], np.float32)
    p = np.asarray(inp["p"], np.float32)
    NB, T = cfg.NB, cfg.T
    in_maps = []
    for c in range(n_cores):
        m = dict(shared)
        m["x"] = np.ascontiguousarray(x[c * NB:(c + 1) * NB].reshape(NB * T, D))
        m["p"] = np.ascontiguousarray(p[:, c * NB:(c + 1) * NB].reshape(cfg.L, NB * T, PLE))
        in_maps.append(m)
    res = run_bass_kernel_spmd(nc, in_maps, core_ids=list(range(n_cores)))
    out = np.concatenate([res.results[c]["y"].reshape(NB, T, D) for c in range(n_cores)], axis=0)
    return out.astype(np.float32)


def kernel(**inputs):
    cfg = Cfg(T=2048, NB=4, E=32, L=4)
    return run(cfg, inputs, 8)
```

```python
import numpy as np
from contextlib import ExitStack
import concourse.bass as bass
import concourse.mybir as mybir
from concourse.bass_utils import run_bass_kernel_spmd

F32 = mybir.dt.float32
BF16 = mybir.dt.bfloat16
ALU = mybir.AluOpType
AF = mybir.ActivationFunctionType

D = 1024
KC = 8
P = 128
HS = 64
PLE = 256
LN_EPS = 1e-5
GN_EPS = 64 * 1e-5
TOPK = 4


class Cfg:
    def __init__(self, T=2048, NB=4, E=32, L=4):
        self.T, self.NB, self.E, self.L = T, NB, E, L
        self.NTOK = T * NB
        self.alpha = (2.0 * L) ** 0.25
        self.NR = (L + 1) // 2
        self.NCV = L // 2


class Sem:
    def __init__(self, h, name):
        self.h, self.name, self.total = h, name, 0


class Buf:
    __slots__ = ("name", "w", "r", "sem", "dram")

    def __init__(self, name, dram=False):
        self.name, self.w, self.r, self.sem, self.dram = name, None, [], None, dram


class Eng:
    def __init__(self, key, sem):
        self.key, self.sem, self.count, self.waited, self.ops = key, sem, 0, {}, []


class Sched:
    def __init__(self, nc, stack, nsem=100):
        self.nc = nc
        self.sems = [Sem(stack.enter_context(nc.semaphore(f"s{i}")), f"s{i}") for i in range(nsem)]
        self.free = list(self.sems)
        self.eng = {k: Eng(k, self.free.pop()) for k in ("pe", "dve", "act", "pool", "sp")}
        self.dmabufs = []

    def _deps(self, e, reads, writes, dma_sem=None):
        deps = {}

        def add(d):
            if d is None:
                return
            s, c = d
            if (s is e.sem and e.key == "pe") or s is dma_sem:
                return
            if deps.get(s, 0) < c:
                deps[s] = c

        for b in reads:
            add(b.w)
        for b in writes:
            if b.dram:
                continue
            add(b.w)
            for d in b.r:
                add(d)
        out = []
        for s, c in deps.items():
            if e.waited.get(s, 0) < c:
                e.waited[s] = c
                out.append((s.h, c))
        return out

    def op(self, ek, fn, reads=(), writes=()):
        e = self.eng[ek]
        waits = self._deps(e, reads, writes)
        e.count += 1
        sem_h = e.sem.h

        def thunk(h):
            for s, c in waits:
                h.wait_ge(s, c)
            fn(h).then_inc(sem_h, 1)

        e.ops.append(thunk)
        me = (e.sem, e.count)
        for b in reads:
            b.r = [d for d in b.r if d[0] is not e.sem] + [me]
        for b in writes:
            b.w = me
            b.r = []

    def dma(self, qk, out_ap, in_ap, reads=(), writes=()):
        e = self.eng[qk]
        b = writes[0]
        if b.sem is None:
            b.sem = self.free.pop()
            self.dmabufs.append(b)
        waits = self._deps(e, reads, writes, dma_sem=b.sem)
        b.sem.total += 16
        sem_h = b.sem.h

        def thunk(h):
            for s, c in waits:
                h.wait_ge(s, c)
            h.dma_start(out=out_ap, in_=in_ap).then_inc(sem_h, 16)

        e.ops.append(thunk)
        me = (b.sem, b.sem.total)
        for rb in reads:
            rb.r = [d for d in rb.r if d[0] is not b.sem] + [me]
        for wb in writes:
            wb.w = me
            wb.r = []

    def run_phase(self, name):
        nc = self.nc
        sp = self.eng["sp"]
        fin = []
        for b in self.dmabufs:
            if sp.waited.get(b.sem, 0) < b.sem.total:
                sp.waited[b.sem] = b.sem.total
                fin.append((b.sem.h, b.sem.total))
        for k in ("pe", "dve", "act", "pool"):
            e = self.eng[k]
            if e.count and sp.waited.get(e.sem, 0) < e.count:
                sp.waited[e.sem] = e.count
                fin.append((e.sem.h, e.count))

        def spfin(h):
            for s, c in fin:
                h.wait_ge(s, c)

        sp.ops.append(spfin)
        with nc.Block() as block:
            for k, dec in (("sp", block.sync), ("pe", block.tensor), ("dve", block.vector),
                           ("act", block.scalar), ("pool", block.gpsimd)):
                ops = self.eng[k].ops
                if not ops:
                    continue

                def body(h, ops=ops):
                    for t in ops:
                        t(h)

                dec(body)
        for e in self.eng.values():
            e.ops = []
        for b in self.dmabufs:
            self.free.append(b.sem)
            b.sem, b.w, b.r = None, None, []
        self.dmabufs = []


class Builder:
    def __init__(self, cfg):
        self.cfg = cfg
        self.nc = bass.Bass("TRN2", target_bir_lowering=False)
        self.din = {}
        self.uid = 0

    def inp(self, name, shape, dt=F32):
        t = self.nc.dram_tensor(name, list(shape), dt, kind="ExternalInput").ap()
        self.din[name] = t
        return t

    def un(self, name):
        self.uid += 1
        return f"{name}_{self.uid}"

    def scratch(self, name, shape, dt=F32):
        return self.nc.dram_tensor(name, list(shape), dt, kind="Internal").ap()

    def mm(self, out, lhsT, rhs, start, stop, reads, writes):
        self.S.op("pe", lambda h: h.matmul(out, lhsT, rhs, start=start, stop=stop), reads, writes)

    def tr(self, out, in_, ident, reads, writes):
        self.S.op("pe", lambda h: h.transpose(out, in_, ident), reads, writes)

    def tt(self, ek, out, in0, in1, op, reads, writes):
        self.S.op(ek, lambda h: h.tensor_tensor(out=out, in0=in0, in1=in1, op=op), reads, writes)

    def ts(self, ek, out, in0, s1, s2, op0, op1, reads, writes):
        if op1 is None:
            self.S.op(ek, lambda h: h.tensor_scalar(out=out, in0=in0, scalar1=s1, scalar2=None, op0=op0),
                      reads, writes)
        else:
            self.S.op(ek, lambda h: h.tensor_scalar(out=out, in0=in0, scalar1=s1, scalar2=s2, op0=op0, op1=op1),
                      reads, writes)

    def stt(self, out, in0, scalar, in1, op0, op1, reads, writes):
        self.S.op("dve", lambda h: h.scalar_tensor_tensor(out=out, in0=in0, scalar=scalar, in1=in1,
                                                          op0=op0, op1=op1), reads, writes)

    def act(self, out, in_, func, reads, writes, bias=None, scale=None):
        kw = {}
        if bias is not None:
            kw["bias"] = bias
        if scale is not None:
            kw["scale"] = scale
        self.S.op("act", lambda h: h.activation(out=out, in_=in_, func=func, **kw), reads, writes)

    def cp(self, ek, out, in_, reads, writes):
        if ek == "act":
            self.S.op("act", lambda h: h.copy(out=out, in_=in_), reads, writes)
        else:
            self.S.op(ek, lambda h: h.tensor_copy(out=out, in_=in_), reads, writes)

    def memset(self, ek, ap, val, writes):
        self.S.op(ek, lambda h: h.memset(ap, val), (), writes)

    def ld(self, out, in_, buf, q="sp", extra_reads=()):
        self.S.dma(q, out, in_, extra_reads, [buf])

    def st(self, out, in_, dbuf, rbufs, q="sp"):
        self.S.dma(q, out, in_, rbufs, [dbuf])

    def build(self):
        cfg, nc = self.cfg, self.nc
        NTOK, L, E = cfg.NTOK, cfg.L, cfg.E
        NR, NCV = cfg.NR, cfg.NCV
        x_in = self.inp("x", [NTOK, D])
        p_in = self.inp("p", [L, NTOK, PLE])
        cst = self.inp("cst", [P, 4 * P])
        blk2_in = self.inp("blk2", [P, 2])
        NVR = 6 * 8 + 9 * 8
        vr_in = self.inp("vec_rwkv", [NR, P, NVR])
        vc_in = self.inp("vec_conv", [max(NCV, 1), P, 3 * 8])
        NVL = 7 * 8
        vl_in = self.inp("vec_layer", [L, P, NVL])
        bgu_in = self.inp("b_gu", [L, P, E * 16])
        bdn_in = self.inp("b_down", [L, E, D])
        rb_in = self.inp("router_b", [L, 1, E])
        rw_in = self.inp("router_w", [L, D, E])
        wrkv_in = self.inp("w_rkv", [NR, 3, D, D])
        w1_in = self.inp("w1", [NR, D, 64]); w2_in = self.inp("w2", [NR, 64, D])
        a1_in = self.inp("a1", [NR, D, 64]); a2_in = self.inp("a2", [NR, 64, D])
        g1_in = self.inp("g1", [NR, D, 128]); g2_in = self.inp("g2", [NR, 128, D])
        v1_in = self.inp("v1", [max(NR - 1, 1), D, 32]); v2_in = self.inp("v2", [max(NR - 1, 1), 32, D])
        wo_in = self.inp("w_o", [NR, D, D])
        cwin_in = self.inp("conv_w_in", [max(NCV, 1), D, 3 * D])
        cwout_in = self.inp("conv_w_out", [max(NCV, 1), D, D])
        wgu_in = self.inp("moe_w_gu", [L, E, D, 2 * D])
        wdn_in = self.inp("moe_w_down", [L, E, D, D])
        pproj_in = self.inp("ple_w_proj", [L, PLE, D])
        pgate_in = self.inp("ple_w_gate", [L, D, D])
        y_out = nc.dram_tensor("y", [NTOK, D], F32, kind="ExternalOutput").ap()
        XA = self.scratch("XA", [KC, P, NTOK]); XB = self.scratch("XB", [KC, P, NTOK])
        Rs = self.scratch("Rs", [KC, P, NTOK]); Ws = self.scratch("Ws", [KC, P, NTOK])
        Ks = self.scratch("Ks", [KC, P, NTOK]); As = self.scratch("As", [KC, P, NTOK])
        Bs = self.scratch("Bs", [KC, P, NTOK]); Gs = self.scratch("Gs", [KC, P, NTOK])
        BONs = self.scratch("BONs", [KC, P, NTOK]); VFs = self.scratch("VFs", [KC, P, NTOK])
        Ys = self.scratch("Ys", [KC, P, NTOK])
        Vtok = self.scratch("Vtok", [NTOK, D], BF16)
        FFN = self.scratch("FFN", [NTOK, D])
        self.dr = dict(XA=XA, XB=XB)
        self.y_out = y_out

        with ExitStack() as gs:
            self.S = Sched(nc, gs)
            S = self.S
            ps = gs.enter_context(nc.psum_tensor("ps", [P, 4096], F32))
            self.ps = ps
            self.pb = [Buf(f"bank{i}") for i in range(8)]
            cst_t = gs.enter_context(nc.sbuf_tensor("cst_t", [P, 4 * P], F32))
            blk2f = gs.enter_context(nc.sbuf_tensor("blk2f", [P, 2], F32))
            blk2b = gs.enter_context(nc.sbuf_tensor("blk2b", [P, 2], BF16))
            blkb = gs.enter_context(nc.sbuf_tensor("blkb", [P, P], BF16))
            identb = gs.enter_context(nc.sbuf_tensor("identb", [P, P], BF16))
            self.ident = cst_t[:, 0:P]; self.onesD = cst_t[:, P:2 * P]
            self.blk = cst_t[:, 2 * P:3 * P]; self.blk64 = cst_t[:, 3 * P:4 * P]
            self.blkb, self.blk2b, self.identb = blkb, blk2b, identb
            self.cbuf = Buf("cst")
            self.ld(cst_t[:], cst[:, :], self.cbuf)
            self.ld(blk2f[:], blk2_in[:, :], self.cbuf)
            self.cp("dve", blk2b[:], blk2f[:], [self.cbuf], [self.cbuf])
            self.cp("dve", blkb[:], self.blk, [self.cbuf], [self.cbuf])
            self.cp("dve", identb[:], self.ident, [self.cbuf], [self.cbuf])
            S.run_phase("init")

            self.phase_in(x_in, XA)
            cur, nxt = XA, XB
            stop = getattr(cfg, "stop", 10 ** 9)
            nsub = 0
            for i in range(L):
                j = i // 2
                if nsub >= stop:
                    break
                if i % 2 == 0:
                    self.phase_rwkv_proj(cur, j, vr_in, wrkv_in, w1_in, w2_in, a1_in, a2_in, g1_in, g2_in,
                                         v1_in, v2_in, Rs, Ws, Ks, As, Bs, Gs, BONs, VFs, Vtok)
                    self.phase_scan(Rs, Ws, Ks, As, Bs, Vtok, Ys)
                    self.phase_rwkv_out(cur, nxt, j, i, vr_in, vl_in, wo_in, Ys, Gs, BONs)
                else:
                    self.phase_conv(cur, nxt, j, i, vc_in, vl_in, cwin_in, cwout_in)
                cur, nxt = nxt, cur
                nsub += 1
                if nsub >= stop:
                    break
                self.phase_moe(cur, i, rw_in, rb_in, wgu_in, wdn_in, bgu_in, bdn_in, FFN)
                if getattr(cfg, "debug", None) == "gate":
                    self.end_phase("dbg")
                    return nc
                if getattr(cfg, "debug", None) == "ffn":
                    db = Buf("dbg", dram=True)
                    self.S.dma("sp", y_out[:, :], FFN[:, :], (), [db])
                    self.end_phase("dbg")
                    return nc
                self.phase_moe_epi(cur, nxt, i, vl_in, FFN)
                cur, nxt = nxt, cur
                nsub += 1
                if nsub >= stop:
                    break
                self.phase_ple(cur, nxt, i, vl_in, p_in, pproj_in, pgate_in)
                cur, nxt = nxt, cur
                nsub += 1
            self.phase_out(cur, y_out)
        return nc

    def end_phase(self, name):
        self.S.run_phase(name)
        for b in self.pb:
            b.w, b.r = None, []
        self.cbuf.w, self.cbuf.r = None, []

    def bkn(self, i, n):
        return self.ps[:, i * 512:i * 512 + n]

    def bank(self, i):
        return self.ps[:, i * 512:(i + 1) * 512]

    def phase_in(self, x_in, XA):
        cfg, nc = self.cfg, self.nc
        with ExitStack() as st:
            xt = [st.enter_context(nc.sbuf_tensor(self.un(f"pi_xt{i}"), [P, 4, D], F32)) for i in range(2)]
            xf = [st.enter_context(nc.sbuf_tensor(self.un(f"pi_xf{i}"), [P, KC, 512], F32)) for i in range(2)]
            xtb = [Buf(f"xt{i}") for i in range(2)]
            xfb = [Buf(f"xf{i}") for i in range(2)]
            dst = Buf("XA", dram=True)
            for g in range(cfg.NTOK // 512):
                s = g % 2
                self.ld(xt[s][:], x_in[g * 512:(g + 1) * 512, :].rearrange("(a p) d -> p a d", p=P), xtb[s])
                for c in range(KC):
                    bk = (g * KC + c) % 8
                    for a in range(4):
                        self.tr(self.ps[:, bk * 512 + a * P: bk * 512 + (a + 1) * P],
                                xt[s][:, a, c * P:(c + 1) * P], self.ident, [xtb[s], self.cbuf], [self.pb[bk]])
                    self.cp("dve" if c % 2 == 0 else "act", xf[s][:, c, :], self.bank(bk), [self.pb[bk]], [xfb[s]])
                self.st(XA[:, :, g * 512:(g + 1) * 512].rearrange("c p n -> p c n"), xf[s][:], dst, [xfb[s]])
            self.end_phase("in")

    def phase_out(self, X, y_out):
        cfg, nc = self.cfg, self.nc
        with ExitStack() as st:
            xf = [st.enter_context(nc.sbuf_tensor(self.un(f"po_xf{i}"), [P, KC, 512], F32)) for i in range(2)]
            xt = [st.enter_context(nc.sbuf_tensor(self.un(f"po_xt{i}"), [P, 4, D], F32)) for i in range(2)]
            xtb = [Buf(f"xt{i}") for i in range(2)]
            xfb = [Buf(f"xf{i}") for i in range(2)]
            dst = Buf("Y", dram=True)
            for g in range(cfg.NTOK // 512):
                s = g % 2
                self.ld(xf[s][:], X[:, :, g * 512:(g + 1) * 512].rearrange("c p n -> p c n"), xfb[s])
                for a in range(4):
                    for hf in range(2):
                        bk = (g * 8 + a * 2 + hf) % 8
                        for cc in range(4):
                            c = hf * 4 + cc
                            self.tr(self.ps[:, bk * 512 + cc * P: bk * 512 + (cc + 1) * P],
                                    xf[s][:, c, a * P:(a + 1) * P], self.ident, [xfb[s], self.cbuf], [self.pb[bk]])
                        self.cp("dve" if hf == 0 else "act", xt[s][:, a, hf * 512:(hf + 1) * 512], self.bank(bk),
                                [self.pb[bk]], [xtb[s]])
                self.st(y_out[g * 512:(g + 1) * 512, :].rearrange("(a p) d -> p a d", p=P), xt[s][:], dst, [xtb[s]])
            self.end_phase("out")

    def ln_fm(self, z, zb, N, g_ap, b_ap, tmp, tmpb, bk0, vb):
        sq, mean, rstd = tmp["sq"], tmp["mean"], tmp["rstd"]
        b_mean, b_msq = bk0, bk0 + 1
        self.act(sq[:, :, 0:N], z[:, :, 0:N], AF.Square, [zb], [tmpb["sq"]])
        for c in range(KC):
            self.mm(self.ps[:, b_mean * 512:b_mean * 512 + N], self.onesD, z[:, c, 0:N], c == 0, c == KC - 1,
                    [zb, self.cbuf], [self.pb[b_mean]])
        for c in range(KC):
            self.mm(self.ps[:, b_msq * 512:b_msq * 512 + N], self.onesD, sq[:, c, 0:N], c == 0, c == KC - 1,
                    [tmpb["sq"], self.cbuf], [self.pb[b_msq]])
        pm = self.ps[:, b_mean * 512:b_mean * 512 + N]
        pq = self.ps[:, b_msq * 512:b_msq * 512 + N]
        self.cp("act", mean[:, 0:N], pm, [self.pb[b_mean]], [tmpb["mean"]])
        self.tt("dve", rstd[:, 0:N], mean[:, 0:N], mean[:, 0:N], ALU.mult, [tmpb["mean"]], [tmpb["rstd"]])
        self.tt("dve", rstd[:, 0:N], pq, rstd[:, 0:N], ALU.subtract, [self.pb[b_msq], tmpb["rstd"]], [tmpb["rstd"]])
        self.ts("dve", rstd[:, 0:N], rstd[:, 0:N], 0.0, LN_EPS, ALU.max, ALU.add, [tmpb["rstd"]], [tmpb["rstd"]])
        self.act(rstd[:, 0:N], rstd[:, 0:N], AF.Sqrt, [tmpb["rstd"]], [tmpb["rstd"]])
        self.S.op("dve", lambda h: h.reciprocal(out=rstd[:, 0:N], in_=rstd[:, 0:N]), [tmpb["rstd"]], [tmpb["rstd"]])
        for c in range(KC):
            self.tt("dve", z[:, c, 0:N], z[:, c, 0:N], mean[:, 0:N], ALU.subtract, [zb, tmpb["mean"]], [zb])
            self.tt("dve", z[:, c, 0:N], z[:, c, 0:N], rstd[:, 0:N], ALU.mult, [zb, tmpb["rstd"]], [zb])
            self.act(z[:, c, 0:N], z[:, c, 0:N], AF.Identity, [zb, vb], [zb],
                     bias=b_ap[:, c:c + 1], scale=g_ap[:, c:c + 1])

    def ln_tmp(self, st, N, tag):
        nc = self.nc
        tmp = dict(sq=st.enter_context(nc.sbuf_tensor(self.un(f"{tag}_sq"), [P, KC, N], F32)),
                   mean=st.enter_context(nc.sbuf_tensor(self.un(f"{tag}_mean"), [P, N], F32)),
                   rstd=st.enter_context(nc.sbuf_tensor(self.un(f"{tag}_rstd"), [P, N], F32)))
        tmpb = dict(sq=Buf("sq"), mean=Buf("mean"), rstd=Buf("rstd"))
        return tmp, tmpb

    def wload(self, dst_tile, src_ap, buf, q="pool"):
        self.S.dma(q, dst_tile, src_ap, (), [buf])

    def phase_rwkv_proj(self, X, j, vr_in, wrkv_in, w1_in, w2_in, a1_in, a2_in, g1_in, g2_in, v1_in, v2_in,
                        Rs, Ws, Ks, As, Bs, Gs, BONs, VFs, Vtok):
        cfg, nc = self.cfg, self.nc
        T = cfg.T
        N = 256
        NS = N // P
        with ExitStack() as st:
            sb = lambda name, shape, dt=F32: st.enter_context(nc.sbuf_tensor(self.un(name), shape, dt))
            vec = sb("rp_vec", [P, 6 * 8 + 9 * 8]); vb = Buf("vec")
            self.ld(vec[:], vr_in[j], vb)
            mixv = lambda c: vec[:, c * 8:(c + 1) * 8]
            voff = lambda k: vec[:, 48 + k * 8: 48 + (k + 1) * 8]
            w0v, a0v, v0v, kkv, kav, rkv = (voff(k) for k in range(6))
            omka = sb("rp_omka", [P, 8])
            self.ts("dve", omka[:], kav, -1.0, 1.0, ALU.mult, ALU.add, [vb], [vb])
            wq = sb("rp_wrkv", [P, 3, KC, D], BF16); wqb = Buf("wrkv")
            for c in range(3):
                self.wload(wq[:, c], wrkv_in[j, c].rearrange("(k p) o -> p k o", p=P), wqb)
            w1 = sb("rp_w1", [P, KC, 64], BF16); a1 = sb("rp_a1", [P, KC, 64], BF16)
            g1 = sb("rp_g1", [P, KC, 128], BF16); v1 = sb("rp_v1", [P, KC, 32], BF16)
            w2 = sb("rp_w2", [64, D], BF16); a2 = sb("rp_a2", [64, D], BF16)
            g2 = sb("rp_g2", [P, D], BF16); v2 = sb("rp_v2", [32, D], BF16)
            wsb = Buf("wsmall")
            self.wload(w1[:], w1_in[j].rearrange("(k p) o -> p k o", p=P), wsb)
            self.wload(a1[:], a1_in[j].rearrange("(k p) o -> p k o", p=P), wsb)
            self.wload(g1[:], g1_in[j].rearrange("(k p) o -> p k o", p=P), wsb)
            self.wload(w2[:], w2_in[j], wsb); self.wload(a2[:], a2_in[j], wsb); self.wload(g2[:], g2_in[j], wsb)
            if j > 0:
                self.wload(v1[:], v1_in[j - 1].rearrange("(k p) o -> p k o", p=P), wsb)
                self.wload(v2[:], v2_in[j - 1], wsb)
            xc = [sb(f"rp_x{i}", [P, KC, N]) for i in range(2)]
            xp = [sb(f"rp_xp{i}", [P, KC, N]) for i in range(2)]
            xcb = [Buf("xc0"), Buf("xc1")]; xpb = [Buf("xp0"), Buf("xp1")]
            xx = sb("rp_xx", [P, KC, N]); xxb = Buf("xx")
            xm = [sb(f"rp_xm{c}", [P, KC, N], BF16) for c in range(6)]
            xmb = [Buf(f"xm{c}") for c in range(6)]
            hw = sb("rp_hw", [64, N], BF16); ha = sb("rp_ha", [64, N], BF16)
            hg = sb("rp_hg", [P, N], BF16); hv = sb("rp_hv", [32, N], BF16)
            hb = [Buf("hw"), Buf("ha"), Buf("hg"), Buf("hv")]
            nm = ["r", "k", "v", "dec", "a", "kk", "t1", "t2", "g", "vf", "G", "ig", "gp"]
            tl = {n: [sb(f"rp_{n}{i}", [P, N]) for i in range(2)] for n in nm}
            tb = {n: [Buf(f"{n}{i}") for i in range(2)] for n in nm}
            vtb16 = [sb(f"rp_vt{i}", [P, NS, D], BF16) for i in range(2)]; vtb = [Buf("vt0"), Buf("vt1")]
            vfm = [sb(f"rp_vfm{i}", [P, KC, N], BF16) for i in range(2)]; vfmb = [Buf("vfm0"), Buf("vfm1")]
            cmask = sb("rp_cmask", [P, N]); cmb = Buf("cmask")
            self.memset("dve", cmask[:], 1.0, [cmb])
            self.memset("dve", cmask[:].rearrange("p (a b) -> p a b", b=64)[:, :, 0:1], 0.0, [cmb])
            C0 = float(np.exp(-0.5))
            dsts = {n: Buf(n, dram=True) for n in ("Rs", "Ws", "Ks", "As", "Bs", "Gs", "BONs", "VFs", "Vtok")}
            ngr = cfg.NTOK // N
            bkc = [0]

            def nb():
                bkc[0] = (bkc[0] + 1) % 8
                return bkc[0]

            for g in range(ngr):
                s = g % 2
                n0 = g * N
                seq_start = (n0 % T) == 0
                self.ld(xc[s][:], X[:, :, n0:n0 + N].rearrange("c p n -> p c n"), xcb[s])
                if seq_start:
                    self.memset("dve", xp[s][:, :, 0:1], 0.0, [xpb[s]])
                    self.ld(xp[s][:, :, 1:N], X[:, :, n0:n0 + N - 1].rearrange("c p n -> p c n"), xpb[s])
                else:
                    self.ld(xp[s][:], X[:, :, n0 - 1:n0 + N - 1].rearrange("c p n -> p c n"), xpb[s])
                self.tt("dve", xx[:], xp[s][:], xc[s][:], ALU.subtract, [xpb[s], xcb[s]], [xxb])
                for c in range(6):
                    for k in range(KC):
                        self.stt(xm[c][:, k, :], xx[:, k, :], mixv(c)[:, k:k + 1], xc[s][:, k, :], ALU.mult, ALU.add,
                                 [xxb, xcb[s], vb], [xmb[c]])
                for (hid, w_, c, width, fn) in ((0, w1, 3, 64, AF.Tanh), (1, a1, 4, 64, AF.Identity),
                                                (2, g1, 5, 128, AF.Sigmoid), (3, v1, 2, 32, AF.Identity)):
                    if hid == 3 and j == 0:
                        continue
                    bk = nb()
                    for k in range(KC):
                        self.mm(self.ps[0:width, bk * 512:bk * 512 + N], w_[:, k, :], xm[c][:, k, :], k == 0, k == KC - 1,
                                [wsb, xmb[c]], [self.pb[bk]])
                    dstt = (hw, ha, hg, hv)[hid]
                    self.act(dstt[:], self.ps[0:width, bk * 512:bk * 512 + N], fn, [self.pb[bk]], [hb[hid]])
                for oc in range(KC):
                    o = oc % 2
                    osl = slice(oc * P, (oc + 1) * P)
                    for (c, name) in ((0, "r"), (1, "k"), (2, "v")):
                        bk = nb()
                        for k in range(KC):
                            self.mm(self.bkn(bk, N), wq[:, c, k, osl], xm[c][:, k, :], k == 0, k == KC - 1,
                                    [wqb, xmb[c]], [self.pb[bk]])
                        self.cp("act", tl[name][o][:], self.bkn(bk, N), [self.pb[bk]], [tb[name][o]])
                    bk = nb()
                    self.mm(self.bkn(bk, N), w2[:, osl], hw[:], True, True, [wsb, hb[0]], [self.pb[bk]])
                    self.act(tl["dec"][o][:], self.bkn(bk, N), AF.Sigmoid, [self.pb[bk], vb], [tb["dec"][o]],
                             bias=w0v[:, oc:oc + 1])
                    self.S.op("dve", lambda h, G_=tl["G"][o], d_=tl["dec"][o]: h.tensor_tensor_scan(
                        out=G_[:], data0=cmask[:], data1=d_[:], initial=0.0, op0=ALU.mult, op1=ALU.add),
                        [tb["dec"][o], cmb], [tb["G"][o]])
                    self.tt("dve", tl["gp"][o][:], tl["G"][o][:], tl["dec"][o][:], ALU.subtract,
                            [tb["G"][o], tb["dec"][o]], [tb["gp"][o]])
                    self.act(tl["gp"][o][:], tl["gp"][o][:], AF.Exp, [tb["gp"][o]], [tb["gp"][o]], scale=-C0)
                    self.act(tl["ig"][o][:], tl["G"][o][:], AF.Exp, [tb["G"][o]], [tb["ig"][o]], scale=C0)
                    self.act(tl["dec"][o][:], tl["G"][o][:], AF.Exp, [tb["G"][o]], [tb["dec"][o]], scale=-C0)
                    self.st(Ws[oc, :, n0:n0 + N], tl["dec"][o][:], dsts["Ws"], [tb["dec"][o]])
                    bk = nb()
                    self.mm(self.bkn(bk, N), a2[:, osl], ha[:], True, True, [wsb, hb[1]], [self.pb[bk]])
                    self.act(tl["a"][o][:], self.bkn(bk, N), AF.Sigmoid, [self.pb[bk], vb], [tb["a"][o]],
                             bias=a0v[:, oc:oc + 1])
                    bk = nb()
                    self.mm(self.bkn(bk, N), g2[:, osl], hg[:], True, True, [wsb, hb[2]], [self.pb[bk]])
                    self.cp("act", tl["g"][o][:], self.bkn(bk, N), [self.pb[bk]], [tb["g"][o]])
                    self.st(Gs[oc, :, n0:n0 + N], tl["g"][o][:], dsts["Gs"], [tb["g"][o]])
                    if j == 0:
                        self.st(VFs[oc, :, n0:n0 + N], tl["v"][o][:], dsts["VFs"], [tb["v"][o]])
                    else:
                        self.ld(tl["vf"][o][:], VFs[oc, :, n0:n0 + N], tb["vf"][o])
                        bk = nb()
                        self.mm(self.bkn(bk, N), v2[:, osl], hv[:], True, True, [wsb, hb[3]], [self.pb[bk]])
                        self.act(tl["t1"][o][:], self.bkn(bk, N), AF.Sigmoid, [self.pb[bk], vb], [tb["t1"][o]],
                                 bias=v0v[:, oc:oc + 1])
                        self.tt("dve", tl["vf"][o][:], tl["vf"][o][:], tl["v"][o][:], ALU.subtract,
                                [tb["vf"][o], tb["v"][o]], [tb["vf"][o]])
                        self.tt("dve", tl["vf"][o][:], tl["vf"][o][:], tl["t1"][o][:], ALU.mult,
                                [tb["vf"][o], tb["t1"][o]], [tb["vf"][o]])
                        self.tt("dve", tl["v"][o][:], tl["v"][o][:], tl["vf"][o][:], ALU.add,
                                [tb["vf"][o], tb["v"][o]], [tb["v"][o]])
                    self.cp("dve", vfm[s][:, oc, :], tl["v"][o][:], [tb["v"][o]], [vfmb[s]])
                    self.ts("dve", tl["kk"][o][:], tl["k"][o][:], kkv[:, oc:oc + 1], None, ALU.mult, None,
                            [tb["k"][o], vb], [tb["kk"][o]])
                    self.act(tl["t1"][o][:], tl["kk"][o][:], AF.Square, [tb["kk"][o]], [tb["t1"][o]])
                    bk = nb()
                    self.mm(self.bkn(bk, N), self.blk, tl["t1"][o][:], True, True, [tb["t1"][o], self.cbuf], [self.pb[bk]])
                    self.act(tl["t1"][o][:], self.bkn(bk, N), AF.Sqrt, [self.pb[bk]], [tb["t1"][o]])
                    self.ts("dve", tl["t1"][o][:], tl["t1"][o][:], 1e-12, None, ALU.max, None, [tb["t1"][o]], [tb["t1"][o]])
                    self.S.op("dve", lambda h, t=tl["t1"][o]: h.reciprocal(out=t[:], in_=t[:]), [tb["t1"][o]], [tb["t1"][o]])
                    self.tt("dve", tl["kk"][o][:], tl["kk"][o][:], tl["t1"][o][:], ALU.mult,
                            [tb["kk"][o], tb["t1"][o]], [tb["kk"][o]])
                    self.tt("dve", tl["t2"][o][:], tl["kk"][o][:], tl["a"][o][:], ALU.mult,
                            [tb["kk"][o], tb["a"][o]], [tb["t2"][o]])
                    self.tt("dve", tl["t2"][o][:], tl["t2"][o][:], tl["ig"][o][:], ALU.mult,
                            [tb["t2"][o], tb["ig"][o]], [tb["t2"][o]])
                    self.st(Bs[oc, :, n0:n0 + N], tl["t2"][o][:], dsts["Bs"], [tb["t2"][o]])
                    self.stt(tl["kk"][o][:], tl["kk"][o][:], -1.0, tl["gp"][o][:], ALU.mult, ALU.mult,
                             [tb["kk"][o], tb["gp"][o]], [tb["kk"][o]])
                    self.st(As[oc, :, n0:n0 + N], tl["kk"][o][:], dsts["As"], [tb["kk"][o]])
                    self.ts("dve", tl["a"][o][:], tl["a"][o][:], kav[:, oc:oc + 1], omka[:, oc:oc + 1], ALU.mult, ALU.add,
                            [tb["a"][o], vb], [tb["a"][o]])
                    self.tt("dve", tl["k"][o][:], tl["k"][o][:], tl["a"][o][:], ALU.mult, [tb["k"][o], tb["a"][o]], [tb["k"][o]])
                    self.stt(tl["t1"][o][:], tl["r"][o][:], rkv[:, oc:oc + 1], tl["k"][o][:], ALU.mult, ALU.mult,
                             [tb["r"][o], tb["k"][o], vb], [tb["t1"][o]])
                    bk = nb()
                    self.mm(self.bkn(bk, N), self.blk, tl["t1"][o][:], True, True, [tb["t1"][o], self.cbuf], [self.pb[bk]])
                    self.tt("dve", tl["t1"][o][:], self.bkn(bk, N), tl["v"][o][:], ALU.mult, [self.pb[bk], tb["v"][o]], [tb["t1"][o]])
                    self.st(BONs[oc, :, n0:n0 + N], tl["t1"][o][:], dsts["BONs"], [tb["t1"][o]])
                    self.tt("dve", tl["k"][o][:], tl["k"][o][:], tl["ig"][o][:], ALU.mult, [tb["k"][o], tb["ig"][o]], [tb["k"][o]])
                    self.st(Ks[oc, :, n0:n0 + N], tl["k"][o][:], dsts["Ks"], [tb["k"][o]])
                    self.tt("dve", tl["r"][o][:], tl["r"][o][:], tl["dec"][o][:], ALU.mult, [tb["r"][o], tb["dec"][o]], [tb["r"][o]])
                    self.st(Rs[oc, :, n0:n0 + N], tl["r"][o][:], dsts["Rs"], [tb["r"][o]])
                for a in range(NS):
                    for hf in range(2):
                        bk = nb()
                        pbf = self.bank(bk).bitcast(BF16)
                        for cc in range(4):
                            c = hf * 4 + cc
                            self.tr(pbf[:, cc * P:(cc + 1) * P], vfm[s][:, c, a * P:(a + 1) * P], self.identb[:],
                                    [vfmb[s], self.cbuf], [self.pb[bk]])
                        self.cp("act", vtb16[s][:, a, hf * 512:(hf + 1) * 512], pbf[:, 0:512], [self.pb[bk]], [vtb[s]])
                self.st(Vtok[n0:n0 + N, :].rearrange("(a p) d -> p a d", p=P), vtb16[s][:], dsts["Vtok"], [vtb[s]])
            self.end_phase("rwkv_proj")

    def phase_scan(self, Rs, Ws, Ks, As, Bs, Vtok, Ys):
        cfg, nc = self.cfg, self.nc
        T, NB = cfg.T, cfg.NB
        TC = 64
        YG = 16
        with ExitStack() as st:
            sb = lambda name, shape, dt=F32: st.enter_context(nc.sbuf_tensor(self.un(name), shape, dt))
            par = {n: [sb(f"sc_{n}{i}", [P, NB * KC, TC]) for i in range(2)] for n in ("r", "w", "k", "a", "b")}
            parb = {n: [Buf(f"{n}0"), Buf(f"{n}1")] for n in par}
            src = dict(r=Rs, w=Ws, k=Ks, a=As, b=Bs)
            vt = [sb(f"sc_vt{i}", [P, NB, D], BF16) for i in range(2)]; vtb = [Buf("vt0"), Buf("vt1")]
            Sb = [self.pb[b] for b in range(NB)]
            Xa = [sb(f"sc_Xa{b}", [P, 512], BF16) for b in range(NB)]; Xab = [Buf(f"Xa{b}") for b in range(NB)]
            Xr = [sb(f"sc_Xr{b}", [P, 512], BF16) for b in range(NB)]; Xrb = [Buf(f"Xr{b}") for b in range(NB)]
            T2 = [sb(f"sc_T2{b}", [P, 512], BF16) for b in range(NB)]; T2b = [Buf(f"T2{b}") for b in range(NB)]
            KV = [sb(f"sc_KV{b}", [P, 512], BF16) for b in range(NB)]; KVb = [Buf(f"KV{b}") for b in range(NB)]
            rsc = [sb(f"sc_rsc{i}", [P, 512]) for i in range(2)]; rscb = [Buf("rsc0"), Buf("rsc1")]
            zero = sb("sc_zero", [P, 512], BF16); zb = Buf("zero")
            sel = sb("sc_sel", [P, P, HS], BF16); selb = Buf("sel")
            yst = [sb(f"sc_y{i}", [P, NB * 4 * 2, YG]) for i in range(2)]; ystb = [Buf("y0"), Buf("y1")]
            ydst = Buf("Ys", dram=True)
            self.cp("dve", sel[:], self.identb[:].unsqueeze(2).to_broadcast([P, P, HS]), [selb, self.cbuf], [selb])
            self.memset("dve", zero[:], 0.0, [zb])
            for b in range(NB):
                self.mm(self.bank(b), self.identb[:], zero[:], True, True, [zb, self.cbuf], [Sb[b]])
            v3p = lambda bk: self.bank(bk).rearrange("p (c i) -> p c i", i=HS)
            v3 = lambda tile: tile[:].rearrange("p (c i) -> p c i", i=HS)
            VBK, YBK = 6, 7
            for t in range(T):
                ci, tl = divmod(t, TC)
                cs = ci % 2
                if tl == 0:
                    for n in par:
                        for b in range(NB):
                            self.ld(par[n][cs][:, b * KC:(b + 1) * KC, :],
                                    src[n][:, :, b * T + t: b * T + t + TC].rearrange("c p n -> p c n"), parb[n][cs])
                vi, tv = divmod(t, P)
                vs = vi % 2
                if tv == 0:
                    for b in range(NB):
                        self.ld(vt[vs][:, b, :], Vtok[b * T + t: b * T + t + P, :], vtb[vs])
                yi, ty = divmod(t, YG)
                bc = lambda n, b: par[n][cs][:, b * KC:(b + 1) * KC, tl].unsqueeze(2).to_broadcast([P, KC, HS])
                for b in range(NB):
                    self.tt("dve", v3(Xa[b]), v3p(b), bc("a", b), ALU.mult, [Sb[b], parb["a"][cs]], [Xab[b]])
                def pe_front(b):
                    sab = 4 + (b % 2)
                    self.mm(self.bank(sab), self.blkb[:], Xa[b][:], True, True, [Xab[b], self.cbuf], [self.pb[sab]])
                    for pl in range(2):
                        rhs = vt[vs][:, b, :].rearrange("p (c l i) -> p c l i", l=2, i=HS)[:, :, pl, :]
                        self.mm(self.ps[pl * HS:(pl + 1) * HS, VBK * 512:(VBK + 1) * 512], sel[:, tv, :], rhs, True, True,
                                [vtb[vs], selb], [self.pb[VBK]])

                self.mm(self.bank(4), self.blkb[:], Xa[0][:], True, True, [Xab[0], self.cbuf], [self.pb[4]])
                for b in range(NB):
                    sab = 4 + (b % 2)
                    if b == 0:
                        for pl in range(2):
                            rhs = vt[vs][:, b, :].rearrange("p (c l i) -> p c l i", l=2, i=HS)[:, :, pl, :]
                            self.mm(self.ps[pl * HS:(pl + 1) * HS, VBK * 512:(VBK + 1) * 512], sel[:, tv, :], rhs, True, True,
                                    [vtb[vs], selb], [self.pb[VBK]])
                    self.tt("dve", v3(KV[b]), v3p(VBK), bc("k", b), ALU.mult, [self.pb[VBK], parb["k"][cs]], [KVb[b]])
                    if b + 1 < NB:
                        pe_front(b + 1)
                    self.tt("dve", v3(T2[b]), v3p(sab), bc("b", b), ALU.mult, [self.pb[sab], parb["b"][cs]], [T2b[b]])
                    self.S.op("pe", lambda h, o=self.bank(b), r_=T2[b][:]: h.matmul(o, self.identb[:], r_, start=False, stop=True,
                                                                                     skip_group_check=True),
                              [T2b[b], self.cbuf], [Sb[b]])
                    self.S.op("pe", lambda h, o=self.bank(b), r_=KV[b][:]: h.matmul(o, self.identb[:], r_, start=False, stop=True,
                                                                                     skip_group_check=True),
                              [KVb[b], self.cbuf], [Sb[b]])
                for b in range(NB):
                    self.tt("dve", v3(Xr[b]), v3p(b), bc("r", b), ALU.mult, [Sb[b], parb["r"][cs]], [Xrb[b]])
                    for q in range(4):
                        col = YBK * 512 + ty * (NB * 8) + b * 8 + q * 2
                        self.mm(self.ps[:, col:col + 2], Xr[b][:, q * P:(q + 1) * P], self.blk2b[:], True, True,
                                [Xrb[b], self.cbuf], [self.pb[YBK]])
                if ty == YG - 1:
                    ys = yi % 2
                    self.cp("act", yst[ys][:].rearrange("p c a -> p a c"),
                            self.ps[:, YBK * 512: YBK * 512 + YG * NB * 8].rearrange("p (a c) -> p a c", a=YG),
                            [self.pb[YBK]], [ystb[ys]])
                    t0 = t - YG + 1
                    for b in range(NB):
                        self.st(Ys[:, :, b * T + t0: b * T + t0 + YG].rearrange("c p n -> p c n"),
                                yst[ys][:, b * 8:(b + 1) * 8, :], ydst, [ystb[ys]], q="pool")
                if tl == TC - 1 and t != T - 1:
                    for b in range(NB):
                        rs_ = b % 2
                        self.tt("dve", v3(rsc[rs_]), v3p(b), bc("w", b), ALU.mult, [Sb[b], parb["w"][cs]], [rscb[rs_]])
                        self.mm(self.bank(b), self.ident, rsc[rs_][:], True, True, [rscb[rs_], self.cbuf], [Sb[b]])
            self.end_phase("scan")

    def perm_src(self, A, n0, N, u):
        v = A[:, :, n0:n0 + N].rearrange("(q u) (l i) n -> u i q l n", u=2, l=2)
        return v[u]

    def phase_rwkv_out(self, X, Xn, j, li, vr_in, vl_in, wo_in, Ys, Gs, BONs):
        cfg, nc = self.cfg, self.nc
        N = 256
        with ExitStack() as st:
            sb = lambda name, shape, dt=F32: st.enter_context(nc.sbuf_tensor(self.un(name), shape, dt))
            vec = sb("ro_vec", [P, 15 * 8]); vb = Buf("vec")
            self.ld(vec[:], vr_in[j], vb)
            lg = vec[:, 48 + 6 * 8:48 + 7 * 8]; lb = vec[:, 48 + 7 * 8:48 + 8 * 8]
            vl = sb("ro_vl", [P, 7 * 8])
            self.ld(vl[:], vl_in[li], vb)
            wo = sb("ro_wo", [P, KC, D], BF16); wob = Buf("wo")
            wv = wo_in[j].rearrange("(q u l i) o -> u i q l o", q=4, u=2, l=2)
            for u in range(2):
                for q in range(4):
                    self.wload(wo[u * HS:(u + 1) * HS, q * 2:(q + 1) * 2, :], wv[u][:, q], wob)
            y = [sb(f"ro_y{i}", [P, KC, N]) for i in range(2)]; yb = [Buf("y0"), Buf("y1")]
            gg = [sb(f"ro_g{i}", [P, KC, N]) for i in range(2)]; ggb = [Buf("g0"), Buf("g1")]
            bo = [sb(f"ro_b{i}", [P, KC, N]) for i in range(2)]; bob = [Buf("b0"), Buf("b1")]
            xr = [sb(f"ro_x{i}", [P, KC, N]) for i in range(2)]; xrb = [Buf("x0"), Buf("x1")]
            sq = sb("ro_sq", [P, N]); sqb = Buf("sq")
            mean = sb("ro_mean", [P, N]); meanb = Buf("mean")
            rs = sb("ro_rs", [P, N]); rsb = Buf("rs")
            yg = sb("ro_yg", [P, KC, N], BF16); ygb = Buf("yg")
            tmp, tmpb = self.ln_tmp(st, N, "ro")
            dst = Buf("Xn", dram=True)
            bkc = [0]

            def nb():
                bkc[0] = (bkc[0] + 1) % 6
                return bkc[0]

            for g in range(cfg.NTOK // N):
                s = g % 2
                n0 = g * N
                self.ld(y[s][:], Ys[:, :, n0:n0 + N].rearrange("c p n -> p c n"), yb[s])
                for u in range(2):
                    for q in range(4):
                        self.ld(gg[s][u * HS:(u + 1) * HS, q * 2:(q + 1) * 2, :], self.perm_src(Gs, n0, N, u)[:, q], ggb[s])
                        self.ld(bo[s][u * HS:(u + 1) * HS, q * 2:(q + 1) * 2, :], self.perm_src(BONs, n0, N, u)[:, q], bob[s])
                self.ld(xr[s][:], X[:, :, n0:n0 + N].rearrange("c p n -> p c n"), xrb[s])
                for c in range(KC):
                    bm, bq = nb(), nb()
                    self.act(sq[:], y[s][:, c, :], AF.Square, [yb[s]], [sqb])
                    self.mm(self.bkn(bm, N), self.blk64, y[s][:, c, :], True, True, [yb[s], self.cbuf], [self.pb[bm]])
                    self.mm(self.bkn(bq, N), self.blk64, sq[:], True, True, [sqb, self.cbuf], [self.pb[bq]])
                    self.cp("act", mean[:], self.bkn(bm, N), [self.pb[bm]], [meanb])
                    self.tt("dve", rs[:], mean[:], mean[:], ALU.mult, [meanb], [rsb])
                    self.tt("dve", rs[:], self.bkn(bq, N), rs[:], ALU.subtract, [self.pb[bq], rsb], [rsb])
                    self.ts("dve", rs[:], rs[:], 0.0, GN_EPS, ALU.max, ALU.add, [rsb], [rsb])
                    self.act(rs[:], rs[:], AF.Sqrt, [rsb], [rsb])
                    self.S.op("dve", lambda h: h.reciprocal(out=rs[:], in_=rs[:]), [rsb], [rsb])
                    self.tt("dve", y[s][:, c, :], y[s][:, c, :], mean[:], ALU.subtract, [yb[s], meanb], [yb[s]])
                    self.tt("dve", y[s][:, c, :], y[s][:, c, :], rs[:], ALU.mult, [yb[s], rsb], [yb[s]])
                    self.act(y[s][:, c, :], y[s][:, c, :], AF.Identity, [yb[s], vb], [yb[s]],
                             bias=lb[:, c:c + 1], scale=lg[:, c:c + 1])
                    self.tt("dve", y[s][:, c, :], y[s][:, c, :], bo[s][:, c, :], ALU.add, [yb[s], bob[s]], [yb[s]])
                    self.tt("dve", yg[:, c, :], y[s][:, c, :], gg[s][:, c, :], ALU.mult, [yb[s], ggb[s]], [ygb])
                for oc in range(KC):
                    bk = nb()
                    for k in range(KC):
                        self.mm(self.bkn(bk, N), wo[:, k, oc * P:(oc + 1) * P], yg[:, k, :], k == 0, k == KC - 1,
                                [wob, ygb], [self.pb[bk]])
                    self.stt(xr[s][:, oc, :], xr[s][:, oc, :], cfg.alpha, self.bkn(bk, N), ALU.mult, ALU.add,
                             [xrb[s], self.pb[bk]], [xrb[s]])
                self.ln_fm(xr[s], xrb[s], N, vl[:, 0:8], vl[:, 8:16], tmp, tmpb, 6, vb)
                self.st(Xn[:, :, n0:n0 + N].rearrange("c p n -> p c n"), xr[s][:], dst, [xrb[s]])
            self.end_phase("rwkv_out")

    def phase_conv(self, X, Xn, j, li, vc_in, vl_in, cwin_in, cwout_in):
        cfg, nc = self.cfg, self.nc
        N = 512
        T = cfg.T
        with ExitStack() as st:
            sb = lambda name, shape, dt=F32: st.enter_context(nc.sbuf_tensor(self.un(name), shape, dt))
            vc = sb("cv_vc", [P, 24]); vb = Buf("vec")
            self.ld(vc[:], vc_in[j], vb)
            vl = sb("cv_vl", [P, 56]); self.ld(vl[:], vl_in[li], vb)
            win = sb("cv_win", [P, KC, 3 * D], BF16); winb = Buf("win")
            for k in range(KC):
                self.wload(win[:, k, :], cwin_in[j, k * P:(k + 1) * P, :], winb)
            wout = sb("cv_wout", [P, KC, D], BF16); woutb = Buf("wout")
            self.wload(wout[:], cwout_in[j].rearrange("(k p) o -> p k o", p=P), woutb)
            xr = [sb(f"cv_x{i}", [P, KC, N]) for i in range(2)]; xrb = [Buf("x0"), Buf("x1")]
            xbf = sb("cv_xbf", [P, KC, N], BF16); xbfb = Buf("xbf")
            gb = sb("cv_gb", [P, KC, N], BF16); gbb = Buf("gb")
            gc = sb("cv_gc", [P, N]); gcb = Buf("gc")
            ch = sb("cv_ch", [P, KC, N + 2]); chb = Buf("ch")
            u = sb("cv_u", [P, N]); ub = Buf("u")
            z = sb("cv_z", [P, KC, N], BF16); zb = Buf("z")
            tmp, tmpb = self.ln_tmp(st, N, "cv")
            dst = Buf("Xn", dram=True)
            bkc = [0]

            def nb():
                bkc[0] = (bkc[0] + 1) % 6
                return bkc[0]

            for g in range(cfg.NTOK // N):
                s = g % 2
                n0 = g * N
                self.ld(xr[s][:], X[:, :, n0:n0 + N].rearrange("c p n -> p c n"), xrb[s])
                self.cp("act", xbf[:], xr[s][:], [xrb[s]], [xbfb])
                if (n0 % T) == 0:
                    self.memset("dve", ch[:, :, 0:2], 0.0, [chb])
                else:
                    self.cp("dve", ch[:, :, 0:2], ch[:, :, N:N + 2], [chb], [chb])
                for oc in range(KC):
                    bk = nb()
                    for k in range(KC):
                        self.mm(self.bank(bk), win[:, k, oc * P:(oc + 1) * P], xbf[:, k, :], k == 0, k == KC - 1,
                                [winb, xbfb], [self.pb[bk]])
                    self.cp("act", gb[:, oc, :], self.bank(bk), [self.pb[bk]], [gbb])
                    bk = nb()
                    for k in range(KC):
                        self.mm(self.bank(bk), win[:, k, D + oc * P:D + (oc + 1) * P], xbf[:, k, :], k == 0, k == KC - 1,
                                [winb, xbfb], [self.pb[bk]])
                    self.cp("act", gc[:], self.bank(bk), [self.pb[bk]], [gcb])
                    bk = nb()
                    for k in range(KC):
                        self.mm(self.bank(bk), win[:, k, 2 * D + oc * P:2 * D + (oc + 1) * P], xbf[:, k, :], k == 0, k == KC - 1,
                                [winb, xbfb], [self.pb[bk]])
                    self.tt("dve", ch[:, oc, 2:N + 2], self.bank(bk), gc[:], ALU.mult, [self.pb[bk], gcb], [chb])
                    self.ts("dve", u[:], ch[:, oc, 2:N + 2], vc[:, 16 + oc:17 + oc], None, ALU.mult, None, [chb, vb], [ub])
                    self.stt(u[:], ch[:, oc, 1:N + 1], vc[:, 8 + oc:9 + oc], u[:], ALU.mult, ALU.add, [chb, ub, vb], [ub])
                    self.stt(u[:], ch[:, oc, 0:N], vc[:, oc:oc + 1], u[:], ALU.mult, ALU.add, [chb, ub, vb], [ub])
                    self.tt("dve", z[:, oc, :], u[:], gb[:, oc, :], ALU.mult, [ub, gbb], [zb])
                for oc in range(KC):
                    bk = nb()
                    for k in range(KC):
                        self.mm(self.bank(bk), wout[:, k, oc * P:(oc + 1) * P], z[:, k, :], k == 0, k == KC - 1,
                                [woutb, zb], [self.pb[bk]])
                    self.stt(xr[s][:, oc, :], xr[s][:, oc, :], cfg.alpha, self.bank(bk), ALU.mult, ALU.add,
                             [xrb[s], self.pb[bk]], [xrb[s]])
                self.ln_fm(xr[s], xrb[s], N, vl[:, 0:8], vl[:, 8:16], tmp, tmpb, 6, vb)
                self.st(Xn[:, :, n0:n0 + N].rearrange("c p n -> p c n"), xr[s][:], dst, [xrb[s]])
            self.end_phase("conv")

    def phase_moe(self, X, li, rw_in, rb_in, wgu_in, wdn_in, bgu_in, bdn_in, FFN):
        cfg, nc = self.cfg, self.nc
        E = cfg.E
        NG = 1024 if cfg.NTOK % 1024 == 0 else 512
        NT = NG // P
        with ExitStack() as st:
            sb = lambda name, shape, dt=F32: st.enter_context(nc.sbuf_tensor(self.un(name), shape, dt))
            rw = sb("mo_rw", [P, KC, E]); cb = Buf("const")
            self.ld(rw[:], rw_in[li].rearrange("(k p) e -> p k e", p=P), cb)
            rbias = sb("mo_rb", [1, E]); self.ld(rbias[:], rb_in[li], cb)
            ones1 = sb("mo_ones", [1, P]); self.memset("dve", ones1[:], 1.0, [cb])
            bgu = sb("mo_bgu", [P, E * 16]); self.ld(bgu[:], bgu_in[li], cb)
            bdn = sb("mo_bdn", [E, D]); self.ld(bdn[:], bdn_in[li], cb)
            big = sb("mo_big", [P, NT * D]); bigb = Buf("big")
            xbf = sb("mo_xbf", [P, KC, NG], BF16); xbfb = Buf("xbf")
            gate = sb("mo_gate", [P, NT, E]); gateb = Buf("gate")
            gT = sb("mo_gT", [E, NG]); gTb = Buf("gT")
            lg = sb("mo_lg", [P, E]); lgb = Buf("lg")
            m8 = sb("mo_m8", [P, 8]); nmx = sb("mo_nmx", [P, 1]); msk = sb("mo_msk", [P, E]); ssum = sb("mo_ssum", [P, 1])
            smb = Buf("small")
            wgu = [sb(f"mo_wgu{i}", [P, KC, 2 * D], BF16) for i in range(2)]; wgub = [Buf("wgu0"), Buf("wgu1")]
            wdn = sb("mo_wdn", [P, KC, D], BF16); wdnb = Buf("wdn")
            actt = sb("mo_act", [P, KC, 512], BF16); actb = [Buf(f"act{c}") for c in range(KC)]
            tg = [sb(f"mo_tg{i}", [P, 512]) for i in range(2)]; tgb = [Buf("tg0"), Buf("tg1")]
            tsg = [sb(f"mo_ts{i}", [P, 512]) for i in range(2)]; tsb = [Buf("ts0"), Buf("ts1")]
            tln = [sb(f"mo_tl{i}", [P, 512]) for i in range(2)]; tlb = [Buf("tl0"), Buf("tl1")]
            dst = Buf("FFN", dram=True)
            xv = big[:].rearrange("p (c n) -> p c n", c=KC)
            acc = big[:].rearrange("p (t d) -> p t d", t=NT)
            bkc = [0]

            def nb():
                bkc[0] = (bkc[0] + 1) % 8
                return bkc[0]

            ngroups = cfg.NTOK // NG
            ecount = 0
            for g in range(ngroups):
                n0 = g * NG
                self.ld(xv, X[:, :, n0:n0 + NG].rearrange("c p n -> p c n"), bigb)
                self.cp("act", xbf[:], xv, [bigb], [xbfb])
                for t in range(NT):
                    bk = nb()
                    for k in range(KC):
                        self.mm(self.ps[:, bk * 512:bk * 512 + E], xv[:, k, t * P:(t + 1) * P], rw[:, k, :], k == 0, False,
                                [bigb, cb], [self.pb[bk]])
                    self.mm(self.ps[:, bk * 512:bk * 512 + E], ones1[:], rbias[:], False, True, [cb], [self.pb[bk]])
                    self.cp("dve", lg[:], self.ps[:, bk * 512:bk * 512 + E], [self.pb[bk]], [lgb])
                    self.S.op("dve", lambda h: h.max(out=m8[:], in_=lg[:]), [lgb], [smb])
                    self.ts("dve", msk[:], lg[:], m8[:, TOPK - 1:TOPK], None, ALU.is_ge, None, [lgb, smb], [smb])
                    self.ts("dve", nmx[:], m8[:, 0:1], -1.0, None, ALU.mult, None, [smb], [smb])
                    self.act(lg[:], lg[:], AF.Exp, [lgb, smb], [lgb], bias=nmx[:, 0:1])
                    self.tt("dve", lg[:], lg[:], msk[:], ALU.mult, [lgb, smb], [lgb])
                    self.S.op("dve", lambda h: h.reduce_sum(out=ssum[:], in_=lg[:], axis=mybir.AxisListType.X), [lgb], [smb])
                    self.S.op("dve", lambda h: h.reciprocal(out=ssum[:], in_=ssum[:]), [smb], [smb])
                    self.ts("dve", gate[:, t, :], lg[:], ssum[:, 0:1], None, ALU.mult, None, [lgb, smb], [gateb])
                    bk = nb()
                    self.tr(self.ps[0:E, bk * 512:bk * 512 + P], gate[:, t, :], self.ident, [gateb, self.cbuf], [self.pb[bk]])
                    self.cp("act", gT[:, t * P:(t + 1) * P], self.ps[0:E, bk * 512:bk * 512 + P], [self.pb[bk]], [gTb])
                if getattr(cfg, "debug", None) == "gate":
                    self.S.dma("sp", self.y_out[n0:n0 + NG, 0:E].rearrange("(t p) e -> p t e", p=P), gate[:], [gateb], [dst])
                    self.S.dma("sp", self.y_out[n0:n0 + P, 64:64 + 8], m8[:], [smb], [dst])
                    self.S.dma("sp", self.y_out[n0:n0 + P, 128:128 + E], msk[:], [smb], [dst])
                    self.S.dma("sp", self.y_out[n0:n0 + P, 192:192 + 8], m8[:], [smb], [dst])
                    self.S.dma("sp", self.y_out[n0:n0 + P, 256:256 + E], lg[:], [lgb], [dst])
                for t in range(NT):
                    for hf in range(2):
                        bk = nb()
                        self.mm(self.bank(bk), gT[:, t * P:(t + 1) * P], bdn[:, hf * 512:(hf + 1) * 512], True, True,
                                [gTb, cb], [self.pb[bk]])
                        self.cp("act", acc[:, t, hf * 512:(hf + 1) * 512], self.bank(bk), [self.pb[bk]], [bigb])
                for e in range(E):
                    ws = ecount % 2
                    ecount += 1
                    for k in range(KC):
                        self.wload(wgu[ws][:, k, :], wgu_in[li, e, k * P:(k + 1) * P, :], wgub[ws])
                    self.wload(wdn[:], wdn_in[li, e].rearrange("(k p) o -> p k o", p=P), wdnb)
                    for sub in range(NG // 512):
                        tok = slice(sub * 512, (sub + 1) * 512)
                        for fc in range(KC):
                            o = fc % 2
                            bkg = nb()
                            for k in range(KC):
                                self.mm(self.bank(bkg), wgu[ws][:, k, fc * P:(fc + 1) * P], xbf[:, k, tok], k == 0, k == KC - 1,
                                        [wgub[ws], xbfb], [self.pb[bkg]])
                            bkl = nb()
                            for k in range(KC):
                                self.mm(self.bank(bkl), wgu[ws][:, k, D + fc * P:D + (fc + 1) * P], xbf[:, k, tok], k == 0,
                                        k == KC - 1, [wgub[ws], xbfb], [self.pb[bkl]])
                            bg_ = bgu[:, e * 16 + fc:e * 16 + fc + 1]
                            bl_ = bgu[:, e * 16 + 8 + fc:e * 16 + 8 + fc + 1]
                            self.ts("dve", tg[o][:], self.bank(bkg), bg_, 7.0, ALU.add, ALU.min, [self.pb[bkg], cb], [tgb[o]])
                            self.act(tsg[o][:], tg[o][:], AF.Sigmoid, [tgb[o]], [tsb[o]], scale=1.702)
                            self.ts("dve", tln[o][:], self.bank(bkl), bl_, 7.0, ALU.add, ALU.min, [self.pb[bkl], cb], [tlb[o]])
                            self.ts("dve", tln[o][:], tln[o][:], -7.0, 1.0, ALU.max, ALU.add, [tlb[o]], [tlb[o]])
                            self.tt("dve", tg[o][:], tg[o][:], tsg[o][:], ALU.mult, [tgb[o], tsb[o]], [tgb[o]])
                            self.tt("dve", actt[:, fc, :], tg[o][:], tln[o][:], ALU.mult, [tgb[o], tlb[o]], [actb[fc]])
                        for tt_ in range(4):
                            t = sub * 4 + tt_
                            for hf in range(2):
                                bk = nb()
                                for k in range(KC):
                                    self.mm(self.bank(bk), actt[:, k, tt_ * P:(tt_ + 1) * P], wdn[:, k, hf * 512:(hf + 1) * 512],
                                            k == 0, k == KC - 1, [actb[k], wdnb], [self.pb[bk]])
                                self.stt(acc[:, t, hf * 512:(hf + 1) * 512], self.bank(bk), gate[:, t, e:e + 1],
                                         acc[:, t, hf * 512:(hf + 1) * 512], ALU.mult, ALU.add,
                                         [self.pb[bk], gateb, bigb], [bigb])
                self.st(FFN[n0:n0 + NG, :].rearrange("(t p) d -> p t d", p=P), acc, dst, [bigb])
            self.end_phase("moe")

    def phase_moe_epi(self, X, Xn, li, vl_in, FFN):
        cfg, nc = self.cfg, self.nc
        N = 512
        with ExitStack() as st:
            sb = lambda name, shape, dt=F32: st.enter_context(nc.sbuf_tensor(self.un(name), shape, dt))
            vl = sb("me_vl", [P, 56]); vb = Buf("vec"); self.ld(vl[:], vl_in[li], vb)
            xr = [sb(f"me_x{i}", [P, KC, N]) for i in range(2)]; xrb = [Buf("x0"), Buf("x1")]
            ft = [sb(f"me_f{i}", [P, 4, D]) for i in range(2)]; ftb = [Buf("f0"), Buf("f1")]
            tmp, tmpb = self.ln_tmp(st, N, "me")
            dst = Buf("Xn", dram=True)
            for g in range(cfg.NTOK // N):
                s = g % 2
                n0 = g * N
                self.ld(xr[s][:], X[:, :, n0:n0 + N].rearrange("c p n -> p c n"), xrb[s])
                self.ld(ft[s][:], FFN[n0:n0 + N, :].rearrange("(a p) d -> p a d", p=P), ftb[s])
                for c in range(KC):
                    bk = (g * KC + c) % 6
                    for a in range(4):
                        self.tr(self.ps[:, bk * 512 + a * P: bk * 512 + (a + 1) * P], ft[s][:, a, c * P:(c + 1) * P],
                                self.ident, [ftb[s], self.cbuf], [self.pb[bk]])
                    self.stt(xr[s][:, c, :], xr[s][:, c, :], cfg.alpha, self.bank(bk), ALU.mult, ALU.add,
                             [xrb[s], self.pb[bk]], [xrb[s]])
                self.ln_fm(xr[s], xrb[s], N, vl[:, 16:24], vl[:, 24:32], tmp, tmpb, 6, vb)
                self.st(Xn[:, :, n0:n0 + N].rearrange("c p n -> p c n"), xr[s][:], dst, [xrb[s]])
            self.end_phase("moe_epi")

    def phase_ple(self, X, Xn, li, vl_in, p_in, pproj_in, pgate_in):
        cfg, nc = self.cfg, self.nc
        N = 512
        with ExitStack() as st:
            sb = lambda name, shape, dt=F32: st.enter_context(nc.sbuf_tensor(self.un(name), shape, dt))
            vl = sb("pl_vl", [P, 56]); vb = Buf("vec"); self.ld(vl[:], vl_in[li], vb)
            wp = sb("pl_wp", [P, 2, D], BF16); wg = sb("pl_wg", [P, KC, D], BF16); wb = Buf("w")
            self.wload(wp[:], pproj_in[li].rearrange("(k p) o -> p k o", p=P), wb)
            self.wload(wg[:], pgate_in[li].rearrange("(k p) o -> p k o", p=P), wb)
            xr = [sb(f"pl_x{i}", [P, KC, N]) for i in range(2)]; xrb = [Buf("x0"), Buf("x1")]
            xbf = sb("pl_xbf", [P, KC, N], BF16); xbfb = Buf("xbf")
            pt = [sb(f"pl_p{i}", [P, 4, PLE]) for i in range(2)]; ptb = [Buf("p0"), Buf("p1")]
            pf = sb("pl_pf", [P, 2, N], BF16); pfb = Buf("pf")
            sg = sb("pl_sg", [P, N]); sgb = Buf("sg")
            tmp, tmpb = self.ln_tmp(st, N, "pl")
            dst = Buf("Xn", dram=True)
            bkc = [0]

            def nb():
                bkc[0] = (bkc[0] + 1) % 6
                return bkc[0]

            for g in range(cfg.NTOK // N):
                s = g % 2
                n0 = g * N
                self.ld(xr[s][:], X[:, :, n0:n0 + N].rearrange("c p n -> p c n"), xrb[s])
                self.ld(pt[s][:], p_in[li, n0:n0 + N, :].rearrange("(a p) d -> p a d", p=P), ptb[s])
                self.cp("act", xbf[:], xr[s][:], [xrb[s]], [xbfb])
                for c in range(2):
                    bk = nb()
                    for a in range(4):
                        self.tr(self.ps[:, bk * 512 + a * P: bk * 512 + (a + 1) * P], pt[s][:, a, c * P:(c + 1) * P],
                                self.ident, [ptb[s], self.cbuf], [self.pb[bk]])
                    self.cp("act", pf[:, c, :], self.bank(bk), [self.pb[bk]], [pfb])
                for oc in range(KC):
                    bk = nb()
                    for k in range(KC):
                        self.mm(self.bank(bk), wg[:, k, oc * P:(oc + 1) * P], xbf[:, k, :], k == 0, k == KC - 1,
                                [wb, xbfb], [self.pb[bk]])
                    self.act(sg[:], self.bank(bk), AF.Sigmoid, [self.pb[bk], vb], [sgb], bias=vl[:, 32 + oc:33 + oc])
                    bk = nb()
                    for k in range(2):
                        self.mm(self.bank(bk), wp[:, k, oc * P:(oc + 1) * P], pf[:, k, :], k == 0, k == 1,
                                [wb, pfb], [self.pb[bk]])
                    self.tt("dve", sg[:], self.bank(bk), sg[:], ALU.mult, [self.pb[bk], sgb], [sgb])
                    self.stt(xr[s][:, oc, :], xr[s][:, oc, :], cfg.alpha, sg[:], ALU.mult, ALU.add, [xrb[s], sgb], [xrb[s]])
                self.ln_fm(xr[s], xrb[s], N, vl[:, 40:48], vl[:, 48:56], tmp, tmpb, 6, vb)
                self.st(Xn[:, :, n0:n0 + N].rearrange("c p n -> p c n"), xr[s][:], dst, [xrb[s]])
            self.end_phase("ple")


def _fm(v):
    v = np.asarray(v, np.float32)
    lead = int(np.prod(v.shape[:-1])) if v.ndim > 1 else 1
    return np.ascontiguousarray(v.reshape(lead, KC, P).transpose(2, 0, 1).reshape(P, lead * KC))


def _fm_perm(v):
    v = np.asarray(v, np.float32).reshape(4, 2, 2, HS)
    return np.ascontiguousarray(v.transpose(1, 3, 0, 2).reshape(P, 8))


def make_consts():
    ident = np.eye(P, dtype=np.float32)
    onesD = np.full((P, P), 1.0 / D, np.float32)
    blk = np.zeros((P, P), np.float32)
    blk[:HS, :HS] = 1.0
    blk[HS:, HS:] = 1.0
    cst = np.concatenate([ident, onesD, blk, blk / HS], axis=1)
    blk2 = np.zeros((P, 2), np.float32)
    blk2[:HS, 0] = 1.0
    blk2[HS:, 1] = 1.0
    return np.ascontiguousarray(cst), blk2


def prep_shared(cfg, inp):
    L, E, NR, NCV = cfg.L, cfg.E, cfg.NR, cfg.NCV
    f = lambda k: np.asarray(inp[k], np.float32)
    cst, blk2 = make_consts()
    vr = np.zeros((NR, P, 15 * 8), np.float32)
    for j in range(NR):
        cols = [_fm(f("rwkv_mix")[j])]
        cols.append(_fm(f("rwkv_w0")[j])); cols.append(_fm(f("rwkv_a0")[j]))
        cols.append(_fm(f("rwkv_v0")[j - 1]) if j > 0 else np.zeros((P, 8), np.float32))
        cols.append(_fm(f("rwkv_k_k")[j])); cols.append(_fm(f("rwkv_k_a")[j]))
        cols.append(_fm(f("rwkv_r_k")[j].reshape(D)))
        cols.append(_fm_perm(f("rwkv_lnx_g")[j])); cols.append(_fm_perm(f("rwkv_lnx_b")[j]))
        cols.append(np.zeros((P, 8), np.float32))
        vr[j] = np.concatenate(cols, axis=1)
    vc = np.zeros((max(NCV, 1), P, 24), np.float32)
    for j in range(NCV):
        vc[j] = _fm(f("conv_w")[j])
    vl = np.zeros((L, P, 56), np.float32)
    for i in range(L):
        vl[i] = np.concatenate([_fm(f(k)[i]) for k in ("ln_mix_g", "ln_mix_b", "ln_ffn_g", "ln_ffn_b", "ple_b_gate",
                                                       "ln_ple_g", "ln_ple_b")], axis=1)
    bgu = np.stack([np.ascontiguousarray(f("moe_b_gu")[i].reshape(E, 16, P).transpose(2, 0, 1).reshape(P, E * 16))
                    for i in range(L)])
    d = dict(cst=cst, blk2=blk2, vec_rwkv=vr, vec_conv=vc, vec_layer=vl, b_gu=bgu,
             b_down=f("moe_b_down"), router_b=f("router_b").reshape(L, 1, E), router_w=f("router_w"),
             w_rkv=f("rwkv_w_rkv"), w1=f("rwkv_w1"), w2=f("rwkv_w2"), a1=f("rwkv_a1"), a2=f("rwkv_a2"),
             g1=f("rwkv_g1"), g2=f("rwkv_g2"), w_o=f("rwkv_w_o"),
             conv_w_in=f("conv_w_in"), conv_w_out=f("conv_w_out"),
             moe_w_gu=f("moe_w_gu"), moe_w_down=f("moe_w_down"),
             ple_w_proj=f("ple_w_proj"), ple_w_gate=f("ple_w_gate"))
    if NR > 1:
        d["v1"] = f("rwkv_v1"); d["v2"] = f("rwkv_v2")
    else:
        d["v1"] = np.zeros((1, D, 32), np.float32); d["v2"] = np.zeros((1, 32, D), np.float32)
    if NCV == 0:
        d["conv_w_in"] = np.zeros((1, D, 3 * D), np.float32); d["conv_w_out"] = np.zeros((1, D, D), np.float32)
    return d


_NC_CACHE = {}


def run(cfg, inp, n_cores):
    key = (cfg.T, cfg.NB, cfg.E, cfg.L, getattr(cfg, 'stop', None), getattr(cfg, 'debug', None))
    if key not in _NC_CACHE:
        _NC_CACHE[key] = Builder(cfg).build()
    nc = _NC_CACHE[key]
    shared = prep_shared(cfg, inp)
    x = np.asarray(inp["x"], np.float32)
    p = np.asarray(inp["p"], np.float32)
    NB, T = cfg.NB, cfg.T
    in_maps = []
    for c in range(n_cores):
        m = dict(shared)
        m["x"] = np.ascontiguousarray(x[c * NB:(c + 1) * NB].reshape(NB * T, D))
        m["p"] = np.ascontiguousarray(p[:, c * NB:(c + 1) * NB].reshape(cfg.L, NB * T, PLE))
        in_maps.append(m)
    res = run_bass_kernel_spmd(nc, in_maps, core_ids=list(range(n_cores)))
    out = np.concatenate([res.results[c]["y"].reshape(NB, T, D) for c in range(n_cores)], axis=0)
    return out.astype(np.float32)


def kernel(**inputs):
    cfg = Cfg(T=2048, NB=4, E=32, L=4)
    return run(cfg, inputs, 8)
```
